# Optimizing a Trainium2 kernel written in Bass

```python
import math
import jax, jax.numpy as jnp
from jax import lax
import numpy as np

D_MODEL = 2048
BATCH = 4
SEQ = 4096
DEPTH = 2

N_BRANCH = 4
BRANCH_WIDTH = D_MODEL // N_BRANCH
NORM_EPS = 1e-6
CHUNK = 64
GDN_HEADS = 4
GDN_HEAD_DIM = BRANCH_WIDTH // GDN_HEADS
GDN_CONV = 4
RWKV_HEAD_DIM = 64
RWKV_HEADS = BRANCH_WIDTH // RWKV_HEAD_DIM
RWKV_DECAY_LORA = 64
RWKV_ICLR_LORA = 64
RWKV_GATE_LORA = 128
RWKV_DECAY_SCALE = 0.606531
RWKV_LN_EPS = 64e-5
RWKV_WIDTHS = (BRANCH_WIDTH, BRANCH_WIDTH, BRANCH_WIDTH, RWKV_DECAY_LORA, RWKV_ICLR_LORA, RWKV_GATE_LORA)
RWKV_IN = sum(RWKV_WIDTHS)
RWKV_SPLITS = tuple(sum(RWKV_WIDTHS[:i + 1]) for i in range(len(RWKV_WIDTHS) - 1))
POOL_GROUPS = 4
POOL_GROUP_WIDTH = BRANCH_WIDTH // POOL_GROUPS
POOL_WINDOWS = (2, 4, 8, 16)
POOL_MAX_WINDOW = 16
GLA_HEADS = 4
GLA_KEY_DIM = BRANCH_WIDTH // 2 // GLA_HEADS
GLA_VAL_DIM = BRANCH_WIDTH // GLA_HEADS
GLA_GATE_RANK = 16
GLA_GATE_NORM = 16.0
IN_WIDTHS = (3 * BRANCH_WIDTH,
             BRANCH_WIDTH,
             GDN_HEADS,
             GDN_HEADS,
             RWKV_IN,
             BRANCH_WIDTH,
             GLA_HEADS * GLA_KEY_DIM,
             GLA_HEADS * GLA_KEY_DIM,
             BRANCH_WIDTH,
             BRANCH_WIDTH,
             GLA_GATE_RANK,
             N_BRANCH * D_MODEL)
IN_WIDTH = sum(IN_WIDTHS)
IN_SPLITS = tuple(sum(IN_WIDTHS[:i + 1]) for i in range(len(IN_WIDTHS) - 1))
D_FF = 5632
N_EXPERTS = 8
TOP_K = 2
D_FF_EXPERT = 7168
MOE_BLOCK = 512

kernel_name = 'hybrid_gdn_rwkv7_pool_gla_moe_trunk'

F32 = jnp.float32


def rmsnorm(x, g, eps=NORM_EPS):
    xf = x.astype(F32)
    y = xf * lax.rsqrt(jnp.mean(xf * xf, axis=-1, keepdims=True) + eps)
    return (y * g.astype(F32)).astype(x.dtype)


def l2norm(x, eps=1e-6):
    return x * lax.rsqrt(jnp.sum(x * x, axis=-1, keepdims=True) + eps)


def causal_depthwise_conv(x, w):
    width, ch = w.shape
    return lax.conv_general_dilated(x, w[:, None, :].astype(x.dtype), window_strides=(1,),
                                    padding=((width - 1, 0),),
                                    dimension_numbers=('NWC', 'WIO', 'NWC'),
                                    feature_group_count=ch)


def to_chunks(t):
    b, s = t.shape[:2]
    t = t.reshape((b, s // CHUNK, CHUNK) + t.shape[2:])
    return jnp.moveaxis(t, 2, 3)


def from_chunks(t):
    t = jnp.moveaxis(t, 3, 2)
    b, n, c = t.shape[:3]
    return t.reshape((b, n * c) + t.shape[3:])


def chunk_gated_delta_rule(q, k, v, beta, g):
    dk, dv = q.shape[-1], v.shape[-1]
    qc, kc, vc = to_chunks(q * dk ** -0.5), to_chunks(k), to_chunks(v)
    bc = to_chunks(beta)
    gc = jnp.cumsum(to_chunks(g), axis=-1)
    causal = jnp.tril(jnp.ones((CHUNK, CHUNK), bool))
    strict = jnp.tril(jnp.ones((CHUNK, CHUNK), bool), -1)
    diff = gc[..., :, None] - gc[..., None, :]
    decay = jnp.where(causal, jnp.exp(jnp.where(causal, diff, 0.0)), 0.0)
    kb = kc * bc[..., None]
    lower = jnp.where(strict, jnp.einsum('bnhid,bnhjd->bnhij', kb, kc) * decay, 0.0)
    rhs = jnp.concatenate([vc * bc[..., None], kb * jnp.exp(gc)[..., None]], axis=-1)
    sol = lax.linalg.triangular_solve(jnp.eye(CHUNK, dtype=F32) + lower, rhs,
                                      left_side=True, lower=True)
    u, w = sol[..., :dv], sol[..., dv:]
    attn = jnp.einsum('bnhid,bnhjd->bnhij', qc, kc) * decay
    q_dec = qc * jnp.exp(gc)[..., None]
    k_tail = kc * jnp.exp(gc[..., -1:] - gc)[..., None]
    chunk_decay = jnp.exp(gc[..., -1])

    def step(state, inp):
        u_n, w_n, a_n, qd_n, kt_n, cd_n = inp
        v_new = u_n - jnp.einsum('bhik,bhkv->bhiv', w_n, state)
        o = jnp.einsum('bhik,bhkv->bhiv', qd_n, state) + jnp.einsum('bhij,bhjv->bhiv', a_n, v_new)
        state = state * cd_n[..., None, None] + jnp.einsum('bhik,bhiv->bhkv', kt_n, v_new)
        return state, o

    b, n, h = qc.shape[:3]
    s0 = jnp.zeros((b, h, dk, dv), F32)
    xs = tuple(jnp.moveaxis(t, 1, 0) for t in (u, w, attn, q_dec, k_tail, chunk_decay))
    _, o = lax.scan(step, s0, xs)
    return from_chunks(jnp.moveaxis(o, 0, 1))


def chunk_gla(q, k, v, log_f):
    dk, dv = q.shape[-1], v.shape[-1]
    qc, kc, vc = to_chunks(q * dk ** -0.5), to_chunks(k), to_chunks(v)
    gc = jnp.cumsum(to_chunks(log_f), axis=-2)
    causal = jnp.tril(jnp.ones((CHUNK, CHUNK), bool))
    q_dec = qc * jnp.exp(gc)
    attn = jnp.einsum('bnhid,bnhjd->bnhij', q_dec, kc * jnp.exp(-gc))
    intra = jnp.einsum('bnhij,bnhjv->bnhiv', jnp.where(causal, attn, 0.0), vc)
    g_last = gc[..., -1:, :]
    k_tail = kc * jnp.exp(g_last - gc)
    chunk_decay = jnp.exp(g_last[..., 0, :])

    def step(state, inp):
        q_n, k_n, v_n, d_n = inp
        o = jnp.einsum('bhik,bhkv->bhiv', q_n, state)
        state = state * d_n[..., None] + jnp.einsum('bhik,bhiv->bhkv', k_n, v_n)
        return state, o

    b, n, h = qc.shape[:3]
    s0 = jnp.zeros((b, h, dk, dv), F32)
    xs = tuple(jnp.moveaxis(t, 1, 0) for t in (q_dec, k_tail, vc, chunk_decay))
    _, inter = lax.scan(step, s0, xs)
    return from_chunks(intra + jnp.moveaxis(inter, 0, 1))


def gated_deltanet_branch(qkv, z, b_logit, a_logit, conv_w, a_log, dt_bias, norm_g):
    bsz, s, _ = qkv.shape
    qkv = jax.nn.silu(causal_depthwise_conv(qkv.astype(F32), conv_w.astype(F32)))
    q, k, v = (t.reshape(bsz, s, GDN_HEADS, GDN_HEAD_DIM) for t in jnp.split(qkv, 3, axis=-1))
    q, k = l2norm(q), l2norm(k)
    beta = jax.nn.sigmoid(b_logit.astype(F32))
    g = -jnp.exp(a_log.astype(F32)) * jax.nn.softplus(a_logit.astype(F32) + dt_bias.astype(F32))
    o = chunk_gated_delta_rule(q, k, v, beta, g)
    gate = jax.nn.silu(z.astype(F32).reshape(bsz, s, GDN_HEADS, GDN_HEAD_DIM))
    o = rmsnorm(o, norm_g) * gate
    return o.reshape(bsz, s, BRANCH_WIDTH).astype(qkv.dtype)


def rwkv7_branch(hr, mu, w0, w_up, a0, a_up, g_up, k_k, k_a, r_k, ln_g, ln_b):
    bsz, s, _ = hr.shape
    hr = hr.astype(F32)
    prev = jnp.pad(hr, ((0, 0), (1, 0), (0, 0)))[:, :-1]
    hs = hr + (prev - hr) * mu.astype(F32)
    r, k, v, wd, ad, gd = jnp.split(hs, RWKV_SPLITS, axis=-1)
    log_w = -RWKV_DECAY_SCALE * jax.nn.sigmoid(w0 + jnp.tanh(wd) @ w_up.astype(F32))
    a_lr = jax.nn.sigmoid(a0 + ad @ a_up.astype(F32))
    gate = jax.nn.sigmoid(gd) @ g_up.astype(F32)
    heads = lambda t: t.reshape(bsz, s, RWKV_HEADS, RWKV_HEAD_DIM)
    kk = l2norm(heads(k * k_k))
    k = k * (1.0 + (a_lr - 1.0) * k_a)
    r_h, k_h, v_h, a_h = heads(r), heads(k), heads(v), heads(a_lr)

    def step(state, inp):
        r_t, w_t, k_t, v_t, kk_t, a_t = inp
        sa = jnp.einsum('bhvk,bhk->bhv', state, -kk_t)
        state = (state * w_t[:, :, None, :] + sa[..., None] * (kk_t * a_t)[:, :, None, :]
                 + v_t[..., None] * k_t[:, :, None, :])
        return state, jnp.einsum('bhvk,bhk->bhv', state, r_t)

    s0 = jnp.zeros((bsz, RWKV_HEADS, RWKV_HEAD_DIM, RWKV_HEAD_DIM), F32)
    xs = tuple(jnp.moveaxis(t, 1, 0) for t in (r_h, jnp.exp(heads(log_w)), k_h, v_h, kk, a_h))
    _, y = lax.scan(step, s0, xs, unroll=8)
    y = jnp.moveaxis(y, 0, 1)
    mean = jnp.mean(y, axis=-1, keepdims=True)
    var = jnp.mean(jnp.square(y - mean), axis=-1, keepdims=True)
    y = ((y - mean) * lax.rsqrt(var + RWKV_LN_EPS)).reshape(bsz, s, BRANCH_WIDTH)
    y = y * ln_g.astype(F32) + ln_b.astype(F32)
    bonus = jnp.sum(r_h * k_h * r_k.astype(F32), axis=-1, keepdims=True) * v_h
    y = (y + bonus.reshape(bsz, s, BRANCH_WIDTH)) * gate
    return y.astype(mu.dtype)


def pool_branch(u, pool_w, pool_scale):
    bsz, s, _ = u.shape
    uf = u.astype(F32).reshape(bsz, s, POOL_GROUPS, POOL_GROUP_WIDTH)
    cs = jnp.cumsum(uf, axis=1)
    cs_pad = jnp.pad(cs, ((0, 0), (POOL_MAX_WINDOW, 0), (0, 0), (0, 0)))
    pos = jnp.arange(s)
    outs = []
    for gi, win in enumerate(POOL_WINDOWS):
        window_sum = cs[:, :, gi] - cs_pad[:, POOL_MAX_WINDOW - win:POOL_MAX_WINDOW - win + s, gi]
        count = jnp.minimum(pos + 1, win).astype(F32)[None, :, None]
        outs.append(window_sum / count - uf[:, :, gi])
    pooled = jnp.stack(outs, axis=2)
    y = jnp.einsum('bsgc,gcd->bsgd', pooled, pool_w.astype(F32)).reshape(bsz, s, BRANCH_WIDTH)
    return (y * pool_scale.astype(F32)).astype(u.dtype)


def gla_branch(q, k, v, g, f_down, f_up, f_bias, norm_g):
    bsz, s, _ = q.shape
    q = q.astype(F32).reshape(bsz, s, GLA_HEADS, GLA_KEY_DIM)
    k = k.astype(F32).reshape(bsz, s, GLA_HEADS, GLA_KEY_DIM)
    v = v.astype(F32).reshape(bsz, s, GLA_HEADS, GLA_VAL_DIM)
    log_f = jax.nn.log_sigmoid(f_down.astype(F32) @ f_up.astype(F32) + f_bias.astype(F32)) / GLA_GATE_NORM
    o = chunk_gla(q, k, v, log_f.reshape(bsz, s, GLA_HEADS, GLA_KEY_DIM))
    o = rmsnorm(o, norm_g) * jax.nn.silu(g.astype(F32).reshape(bsz, s, GLA_HEADS, GLA_VAL_DIM))
    return o.reshape(bsz, s, BRANCH_WIDTH).astype(g.dtype)


def hybrid_mixer(hn, w_in, gdn_conv_w, gdn_a_log, gdn_dt_bias, gdn_norm_g,
                 rwkv_mu, rwkv_w0, rwkv_w_up, rwkv_a0, rwkv_a_up, rwkv_g_up, rwkv_k_k, rwkv_k_a,
                 rwkv_r_k, rwkv_ln_g, rwkv_ln_b, pool_w, pool_scale, gla_f_up, gla_f_bias, gla_norm_g,
                 gate_bias, branch_proj, w_out):
    bsz, s, d = hn.shape
    proj = hn @ w_in
    (gdn_qkv, gdn_z, gdn_b, gdn_a, rwkv_in, pool_in, gla_q, gla_k, gla_v, gla_g, gla_f,
     gate_logits) = jnp.split(proj, IN_SPLITS, axis=-1)
    ys = (
        gated_deltanet_branch(gdn_qkv, gdn_z, gdn_b, gdn_a, gdn_conv_w, gdn_a_log, gdn_dt_bias, gdn_norm_g),
        rwkv7_branch(rwkv_in, rwkv_mu, rwkv_w0, rwkv_w_up, rwkv_a0, rwkv_a_up, rwkv_g_up, rwkv_k_k,
                     rwkv_k_a, rwkv_r_k, rwkv_ln_g, rwkv_ln_b),
        pool_branch(pool_in, pool_w, pool_scale),
        gla_branch(gla_q, gla_k, gla_v, gla_g, gla_f, gla_f_up, gla_f_bias, gla_norm_g),
    )
    gates = jax.nn.sigmoid((gate_logits + gate_bias).astype(F32)).reshape(bsz, s, N_BRANCH, d)
    mixed = gates[:, :, 0] * (ys[0] @ branch_proj[0]).astype(F32)
    for i in range(1, N_BRANCH):
        mixed = mixed + gates[:, :, i] * (ys[i] @ branch_proj[i]).astype(F32)
    return mixed.astype(hn.dtype) @ w_out


def swiglu(h, w1, w3, w2):
    return (jax.nn.silu(h @ w1) * (h @ w3)) @ w2


def moe_swiglu(h, router_w, w1, w3, w2):
    bsz, s, d = h.shape
    t = bsz * s
    n_assign = t * TOP_K
    ht = h.reshape(t, d)
    logits = (ht @ router_w).astype(F32)
    top_logit, top_idx = lax.top_k(logits, TOP_K)
    top_w = jax.nn.softmax(top_logit, axis=-1)
    flat_e = top_idx.reshape(-1)
    flat_tok = jnp.arange(n_assign, dtype=jnp.int32) // TOP_K
    flat_w = top_w.reshape(-1)
    order = jnp.argsort(flat_e)
    sorted_e = flat_e[order]
    counts = jnp.bincount(flat_e, length=N_EXPERTS)
    start = jnp.cumsum(counts) - counts
    padded = (counts + MOE_BLOCK - 1) // MOE_BLOCK * MOE_BLOCK
    pad_end = jnp.cumsum(padded)
    pad_start = pad_end - padded
    dest = pad_start[sorted_e] + (jnp.arange(n_assign) - start[sorted_e])
    n_rows = (-(-n_assign // MOE_BLOCK) + N_EXPERTS) * MOE_BLOCK
    n_blocks = n_rows // MOE_BLOCK
    row_tok = jnp.full((n_rows,), t, jnp.int32).at[dest].set(flat_tok[order])
    row_w = jnp.zeros((n_rows,), F32).at[dest].set(flat_w[order])
    block_e = jnp.minimum(jnp.searchsorted(pad_end, jnp.arange(n_blocks) * MOE_BLOCK, side='right'),
                          N_EXPERTS - 1)
    h_pad = jnp.concatenate([ht, jnp.zeros((1, d), ht.dtype)], axis=0)
    xb = h_pad[row_tok].reshape(n_blocks, MOE_BLOCK, d)

    def expert_block(args):
        x_blk, e = args
        return swiglu(x_blk, w1[e], w3[e], w2[e])

    yb = lax.map(expert_block, (xb, block_e)).reshape(n_rows, d)
    out = jnp.zeros((t + 1, d), F32).at[row_tok].add(yb.astype(F32) * row_w[:, None])
    return out[:t].reshape(bsz, s, d).astype(h.dtype)


def setup_inputs(seed: int = 0) -> dict:
    key = jax.random.key(seed)
    ks = iter(jax.random.split(key, 48))
    nrm = lambda shape, scale: jax.random.normal(next(ks), shape, F32) * scale
    L = DEPTH
    n_dense = (DEPTH + 1) // 2
    n_moe = DEPTH // 2
    dt = jnp.exp(jax.random.uniform(next(ks), (L, GDN_HEADS), F32, math.log(1e-3), math.log(1e-1)))
    return {
        'x': nrm((BATCH, SEQ, D_MODEL), 1.0),
        'norm1_g': 1.0 + nrm((L, D_MODEL), 0.05),
        'w_in': nrm((L, D_MODEL, IN_WIDTH), D_MODEL ** -0.5),
        'gdn_conv_w': nrm((L, GDN_CONV, 3 * BRANCH_WIDTH), GDN_CONV ** -0.5),
        'gdn_a_log': jnp.log(jax.random.uniform(next(ks), (L, GDN_HEADS), F32, 1.0, 16.0)),
        'gdn_dt_bias': dt + jnp.log(-jnp.expm1(-dt)),
        'gdn_norm_g': 1.0 + nrm((L, GDN_HEAD_DIM), 0.05),
        'rwkv_mu': jax.random.uniform(next(ks), (L, RWKV_IN), F32, 0.0, 1.0),
        'rwkv_w0': nrm((L, BRANCH_WIDTH), 0.5),
        'rwkv_w_up': nrm((L, RWKV_DECAY_LORA, BRANCH_WIDTH), RWKV_DECAY_LORA ** -0.5),
        'rwkv_a0': nrm((L, BRANCH_WIDTH), 0.1),
        'rwkv_a_up': nrm((L, RWKV_ICLR_LORA, BRANCH_WIDTH), RWKV_ICLR_LORA ** -0.5),
        'rwkv_g_up': nrm((L, RWKV_GATE_LORA, BRANCH_WIDTH), RWKV_GATE_LORA ** -0.5),
        'rwkv_k_k': 0.85 + nrm((L, BRANCH_WIDTH), 0.05),
        'rwkv_k_a': 1.0 + nrm((L, BRANCH_WIDTH), 0.05),
        'rwkv_r_k': nrm((L, RWKV_HEADS, RWKV_HEAD_DIM), 0.1),
        'rwkv_ln_g': 1.0 + nrm((L, BRANCH_WIDTH), 0.05),
        'rwkv_ln_b': nrm((L, BRANCH_WIDTH), 0.02),
        'pool_w': nrm((L, POOL_GROUPS, POOL_GROUP_WIDTH, POOL_GROUP_WIDTH), POOL_GROUP_WIDTH ** -0.5),
        'pool_scale': 1.0 + nrm((L, BRANCH_WIDTH), 0.1),
        'gla_f_up': nrm((L, GLA_GATE_RANK, GLA_HEADS * GLA_KEY_DIM), GLA_GATE_RANK ** -0.5),
        'gla_f_bias': nrm((L, GLA_HEADS * GLA_KEY_DIM), 0.1),
        'gla_norm_g': 1.0 + nrm((L, GLA_VAL_DIM), 0.05),
        'gate_bias': nrm((L, N_BRANCH * D_MODEL), 0.1),
        'branch_proj': nrm((L, N_BRANCH, BRANCH_WIDTH, D_MODEL), BRANCH_WIDTH ** -0.5),
        'w_out': nrm((L, D_MODEL, D_MODEL), D_MODEL ** -0.5),
        'norm2_g': 1.0 + nrm((L, D_MODEL), 0.05),
        'ffn_w1': nrm((n_dense, D_MODEL, D_FF), D_MODEL ** -0.5),
        'ffn_w3': nrm((n_dense, D_MODEL, D_FF), D_MODEL ** -0.5),
        'ffn_w2': nrm((n_dense, D_FF, D_MODEL), D_FF ** -0.5),
        'moe_router': nrm((n_moe, D_MODEL, N_EXPERTS), D_MODEL ** -0.5),
        'moe_w1': nrm((n_moe, N_EXPERTS, D_MODEL, D_FF_EXPERT), D_MODEL ** -0.5),
        'moe_w3': nrm((n_moe, N_EXPERTS, D_MODEL, D_FF_EXPERT), D_MODEL ** -0.5),
        'moe_w2': nrm((n_moe, N_EXPERTS, D_FF_EXPERT, D_MODEL), D_FF_EXPERT ** -0.5),
        'final_norm_g': 1.0 + nrm((D_MODEL,), 0.05),
    }


def reference(x, norm1_g, w_in, gdn_conv_w, gdn_a_log, gdn_dt_bias, gdn_norm_g,
              rwkv_mu, rwkv_w0, rwkv_w_up, rwkv_a0, rwkv_a_up, rwkv_g_up, rwkv_k_k, rwkv_k_a,
              rwkv_r_k, rwkv_ln_g, rwkv_ln_b, pool_w, pool_scale, gla_f_up, gla_f_bias, gla_norm_g,
              gate_bias, branch_proj, w_out, norm2_g, ffn_w1, ffn_w3, ffn_w2,
              moe_router, moe_w1, moe_w3, moe_w2, final_norm_g):
    h = x
    for layer in range(DEPTH):
        hn = rmsnorm(h, norm1_g[layer])
        h = h + hybrid_mixer(hn, w_in[layer], gdn_conv_w[layer], gdn_a_log[layer], gdn_dt_bias[layer],
                             gdn_norm_g[layer], rwkv_mu[layer], rwkv_w0[layer], rwkv_w_up[layer],
                             rwkv_a0[layer], rwkv_a_up[layer], rwkv_g_up[layer], rwkv_k_k[layer],
                             rwkv_k_a[layer], rwkv_r_k[layer], rwkv_ln_g[layer], rwkv_ln_b[layer],
                             pool_w[layer], pool_scale[layer], gla_f_up[layer], gla_f_bias[layer],
                             gla_norm_g[layer], gate_bias[layer], branch_proj[layer], w_out[layer])
        hn = rmsnorm(h, norm2_g[layer])
        i = layer // 2
        if layer % 2 == 0:
            h = h + swiglu(hn, ffn_w1[i], ffn_w3[i], ffn_w2[i])
        else:
            h = h + moe_swiglu(hn, moe_router[i], moe_w1[i], moe_w3[i], moe_w2[i])
    return rmsnorm(h, final_norm_g)
```

```python
import numpy as np
import concourse.bass as bass
import concourse.mybir as mybir

F32 = mybir.dt.float32
BF16 = mybir.dt.bfloat16
I32 = mybir.dt.int32
U32 = mybir.dt.uint32
AF = mybir.ActivationFunctionType
ALU = mybir.AluOpType
AX = mybir.AxisListType

COMPUTE = ("tensor", "vector", "scalar", "gpsimd")
DMAQ = ("sync", "gpsimd")
NDMASEM = 8


class Op:
    __slots__ = ("eng", "fn", "deps", "is_dma", "idx", "signals", "sigval", "dsem", "dval", "waits", "prewait", "force")

    def __init__(self, eng, fn, is_dma):
        self.eng = eng
        self.fn = fn
        self.is_dma = is_dma
        self.deps = []
        self.signals = False
        self.sigval = None
        self.dsem = None
        self.dval = None
        self.waits = []
        self.prewait = None
        self.force = False


class Prog:
    def __init__(self, nc, same_engine_sync=True):
        self.nc = nc
        self.ops = []
        self.streams = {e: [] for e in ("tensor", "vector", "scalar", "gpsimd", "sync")}
        self.last_w = {}
        self.readers = {}
        self.same_engine_sync = same_engine_sync
        self.ndma = {q: 0 for q in DMAQ}

    def _add(self, eng, fn, reads, writes, is_dma):
        op = Op(eng, fn, is_dma)
        op.idx = len(self.ops)
        reads = list(reads) + ["__ALL__"]
        deps = set()
        for k in reads:
            w = self.last_w.get(k)
            if w is not None:
                deps.add(w)
        for k in writes:
            w = self.last_w.get(k)
            if w is not None:
                deps.add(w)
            for r in self.readers.get(k, ()):
                deps.add(r)
        deps.discard(op.idx)
        op.deps = sorted(deps)
        for k in writes:
            self.last_w[k] = op.idx
            self.readers[k] = []
        for k in reads:
            if k not in writes:
                self.readers.setdefault(k, []).append(op.idx)
        self.ops.append(op)
        self.streams[eng].append(op)
        return op

    def op(self, eng, fn, reads=(), writes=(), force=False):
        o = self._add(eng, fn, list(reads), list(writes), False)
        o.force = force
        return o

    def dma(self, eng, fn, reads=(), writes=()):
        assert eng in DMAQ
        return self._add(eng, fn, list(reads), list(writes), True)

    def emit(self):
        nc = self.nc
        ops = self.ops
        waited = {}
        pos_in_eng = {}
        for e, st in self.streams.items():
            for i, o in enumerate(st):
                pos_in_eng[o.idx] = i
        covered = {e: {} for e in self.streams}
        need = []
        for o in ops:
            e = o.eng
            nw = []
            for d in o.deps:
                do = ops[d]
                if do.is_dma:
                    nw.append(d)
                    continue
                if do.eng == e and not self.same_engine_sync:
                    continue
                if do.eng == e and e == "tensor" and not o.force:
                    continue
                c = covered[e].get(do.eng, -1)
                p = pos_in_eng[d]
                if p > c:
                    covered[e][do.eng] = p
                    nw.append(d)
                    do.signals = True
            need.append(nw)
        sems = {}
        self._sem_ctx = []
        for e in COMPUTE:
            ctx = nc.semaphore("S_" + e)
            s = ctx.__enter__()
            self._sem_ctx.append(ctx)
            sems[e] = s
        dsems = {}
        for q in DMAQ:
            lst = []
            for i in range(NDMASEM):
                ctx = nc.semaphore("D_%s%d" % (q, i))
                lst.append(ctx.__enter__())
                self._sem_ctx.append(ctx)
            dsems[q] = lst
        cnt = {e: 0 for e in COMPUTE}
        dcount = {q: 0 for q in DMAQ}
        for o in ops:
            if o.is_dma:
                j = dcount[o.eng]
                dcount[o.eng] += 1
                o.dsem = dsems[o.eng][j % NDMASEM]
                o.dval = 16 * (j // NDMASEM + 1)
                if j >= NDMASEM:
                    o.prewait = (o.dsem, 16 * (j // NDMASEM))
            elif o.signals:
                cnt[o.eng] += 1
                o.sigval = cnt[o.eng]
        self.maxcnt = dict(cnt)
        dcov = {e: {} for e in self.streams}
        for o, nw in zip(ops, need):
            e = o.eng
            ws = []
            if o.prewait is not None:
                s, v = o.prewait
                if dcov[e].get(id(s), 0) < v:
                    dcov[e][id(s)] = v
                    ws.append((s, v))
            for d in nw:
                do = ops[d]
                if do.is_dma:
                    s, v = do.dsem, do.dval
                    if dcov[e].get(id(s), 0) < v:
                        dcov[e][id(s)] = v
                        ws.append((s, v))
                else:
                    ws.append((sems[do.eng], do.sigval))
            o.waits = ws
        self.sems = sems
        self.dsems = dsems
        finals = {}
        for q in DMAQ:
            n = dcount[q]
            fl = []
            for i in range(min(n, NDMASEM)):
                last_j = ((n - 1 - i) // NDMASEM) * NDMASEM + i
                fl.append((dsems[q][i], 16 * (last_j // NDMASEM + 1)))
            finals[q] = fl

        streams = self.streams

        def run_stream(ename, eng):
            for o in streams[ename]:
                for (s, v) in o.waits:
                    eng.wait_ge(s, v)
                ins = o.fn(eng)
                if o.is_dma:
                    ins.then_inc(o.dsem, 16)
                elif o.signals:
                    ins.then_inc(sems[ename], 1)
            for (s, v) in finals.get(ename, ()):
                eng.wait_ge(s, v)

        with nc.Block() as block:
            @block.tensor
            def _(eng):
                run_stream("tensor", eng)

            @block.vector
            def _(eng):
                run_stream("vector", eng)

            @block.scalar
            def _(eng):
                run_stream("scalar", eng)

            @block.gpsimd
            def _(eng):
                run_stream("gpsimd", eng)

            @block.sync
            def _(eng):
                run_stream("sync", eng)

    def close(self):
        for ctx in reversed(self._sem_ctx):
            ctx.__exit__(None, None, None)

import contextlib
from concourse.bass_utils import run_bass_kernel_spmd

D = 2048
S = 4096
NB = 8
TB = 512
C = 64
EPS = 1e-6
KC = 16
NT_B = 25
NCOLB = NT_B * 128
HALO = 16


class KB:
    def __init__(self):
        self.nc = bass.Bass("TRN2", target_bir_lowering=False)
        self.P = Prog(self.nc)
        self.es = contextlib.ExitStack()
        self.names = set()
        self.manual = set()
        self.bank_base = {}
        self._fence_t = self.sb("fence_t", [128, 8])

    def din(self, name, shape, dtype=F32):
        self.manual.add(name)
        return self.nc.dram_tensor(name, list(shape), dtype, kind="ExternalInput").ap()

    def dout(self, name, shape, dtype=F32):
        self.manual.add(name)
        return self.nc.dram_tensor(name, list(shape), dtype, kind="ExternalOutput").ap()

    def dscr(self, name, shape, dtype=F32):
        self.manual.add(name)
        return self.nc.dram_tensor(name, list(shape), dtype, kind="Internal").ap()

    def sb(self, name, shape, dtype=F32, manual=False):
        assert name not in self.names, name
        self.names.add(name)
        if manual:
            self.manual.add("s_" + name)
        return self.es.enter_context(self.nc.sbuf_tensor("s_" + name, list(shape), dtype))

    def ps(self, name, shape=(128, 512), dtype=F32):
        assert name not in self.names, name
        self.names.add(name)
        return self.es.enter_context(self.nc.psum_tensor("p_" + name, list(shape), dtype))

    def ak(self, aps, extra):
        ks = list(extra)
        for a in aps:
            if a is None or isinstance(a, (int, float)):
                continue
            n = a.tensor.name
            if n in self.manual:
                continue
            ks.append(n)
        return ks

    def _pe_force(self, out, lhsT):
        bank = out.tensor.name
        base = lhsT.base_partition()
        prev = self.bank_base.get(bank)
        self.bank_base[bank] = base
        return prev is not None and prev != base

    def pk(self, aps):
        ks = []
        for a in aps:
            if a is None or isinstance(a, (int, float)):
                continue
            n = a.tensor.name
            if n.startswith("p_"):
                ks.append(n)
        return ks

    def mm(self, out, lhsT, rhs, start=True, stop=True, r=(), w=()):
        f = self._pe_force(out, lhsT)
        self.P.op("tensor", lambda e: e.matmul(out, lhsT, rhs, start=start, stop=stop), reads=self.ak([lhsT, rhs], r), writes=self.ak([out], w), force=f)

    def tr(self, out, in_, ident, r=(), w=()):
        f = self._pe_force(out, in_)
        self.P.op("tensor", lambda e: e.transpose(out, in_, ident), reads=self.ak([in_, ident], r), writes=self.ak([out], w), force=f)

    def act(self, out, in_, func, r=(), w=(), bias=None, scale=None, accum_out=None, eng="scalar"):
        kw = {}
        if bias is not None:
            kw["bias"] = bias
        if scale is not None:
            kw["scale"] = scale
        if accum_out is not None:
            kw["accum_out"] = accum_out
        self.P.op("scalar", lambda e: e.activation(out=out, in_=in_, func=func, **kw), reads=self.ak([in_, bias, scale], r), writes=self.ak([out, accum_out], w) + self.pk([in_]))

    def tt(self, out, in0, in1, op, r=(), w=(), eng="vector"):
        self.P.op(eng, lambda e: e.tensor_tensor(out=out, in0=in0, in1=in1, op=op), reads=self.ak([in0, in1], r), writes=self.ak([out], w) + self.pk([in0, in1]))

    def ts(self, out, in0, s1, op0, s2=None, op1=None, r=(), w=(), eng="vector"):
        r = self.ak([in0, s1, s2], r)
        w = self.ak([out], w) + self.pk([in0])
        if op1 is None:
            self.P.op(eng, lambda e: e.tensor_scalar(out=out, in0=in0, scalar1=s1, scalar2=None, op0=op0), reads=r, writes=w)
        else:
            self.P.op(eng, lambda e: e.tensor_scalar(out=out, in0=in0, scalar1=s1, scalar2=s2, op0=op0, op1=op1), reads=r, writes=w)

    def stt(self, out, in0, scalar, in1, op0, op1, r=(), w=(), accum_out=None):
        if accum_out is None:
            self.P.op("vector", lambda e: e.scalar_tensor_tensor(out=out, in0=in0, scalar=scalar, in1=in1, op0=op0, op1=op1), reads=self.ak([in0, scalar, in1], r), writes=self.ak([out], w) + self.pk([in0, in1]))
        else:
            self.P.op("vector", lambda e: e.scalar_tensor_tensor(out=out, in0=in0, scalar=scalar, in1=in1, op0=op0, op1=op1, accum_out=accum_out), reads=self.ak([in0, scalar, in1], r), writes=self.ak([out, accum_out], w) + self.pk([in0, in1]))

    def cp(self, out, in_, r=(), w=(), eng="vector"):
        r = self.ak([in_], r)
        w = self.ak([out], w) + self.pk([in_])
        if eng == "scalar":
            self.P.op("scalar", lambda e: e.copy(out=out, in_=in_), reads=r, writes=w)
        else:
            self.P.op(eng, lambda e: e.tensor_copy(out=out, in_=in_), reads=r, writes=w)

    def memset(self, ap, val, w=(), eng="vector"):
        self.P.op(eng, lambda e: e.memset(ap, val), writes=self.ak([ap], w))

    def recip(self, out, in_, r=(), w=()):
        self.P.op("vector", lambda e: e.reciprocal(out=out, in_=in_), reads=self.ak([in_], r), writes=self.ak([out], w) + self.pk([in_]))

    def scan(self, out, d0, d1, init, op0, op1, r=(), w=()):
        self.P.op("vector", lambda e: e.tensor_tensor_scan(out=out, data0=d0, data1=d1, initial=init, op0=op0, op1=op1), reads=self.ak([d0, d1, init], r), writes=self.ak([out], w))

    def dma(self, out, in_, r=(), w=(), eng="sync"):
        self.P.dma(eng, lambda e: e.dma_start(out=out, in_=in_), reads=self.ak([in_], r), writes=self.ak([out], w) + self.pk([in_]))

    def fence(self):
        ft = self._fence_t
        self.P.op("vector", lambda e: e.memset(ft[:, :], 0.0), writes=["__ALL__"])

    def phase_begin(self):
        self.es_ph = contextlib.ExitStack()
        self._saved_es = self.es
        self.es = self.es_ph

    def phase_end(self):
        self.fence()
        self.es = self._saved_es
        self.es_ph.close()

    def finish(self):
        self.P.emit()
        self.P.close()
        self.es.close()
        return self.nc


def make_consts():
    c = np.zeros((128, 1024), np.float32)
    c[:, 0:128] = np.eye(128, dtype=np.float32)
    p = np.arange(64)[:, None]
    f = np.arange(64)[None, :]
    c[0:64, 128:192] = (f >= p)
    c[0:64, 192:256] = (f > p)
    c[0:64, 256:320] = (p > f)
    rm = np.ones((128, 512), np.float32)
    rm[:, 0::64] = 0.0
    c[:, 320:832] = rm
    c[:, 832:960] = 1.0
    bo = np.zeros((128, 128), np.float32)
    bo[0:64, 0:64] = 1.0
    bo[64:128, 64:128] = 1.0
    c2 = np.zeros((128, 256), np.float32)
    c2[:, 0:128] = bo
    for gi, win in enumerate((2, 4, 8, 16)):
        c2[:, 128 + gi * 16:128 + (gi + 1) * 16] = 1.0 / np.minimum(np.arange(16) + 1, win)
    return np.concatenate([c, c2], axis=1)

CW = 1280
C_ID, C_UI, C_US, C_LS, C_RM, C_ONE, C_BO, C_PC = 0, 128, 192, 256, 320, 832, 1024, 1152


def b_cols(hh):
    cols = []
    def rng(a, n):
        cols.extend(range(a, a + n))
    for h in (2 * hh, 2 * hh + 1):
        rng(0 + h * 128, 128); rng(512 + h * 128, 128); rng(1024 + h * 128, 128); rng(1536 + h * 128, 128)
    RW = 2056
    for pr in (0, 1):
        h0 = 4 * hh + 2 * pr
        rng(RW + h0 * 64, 128); rng(RW + 512 + h0 * 64, 128); rng(RW + 1024 + h0 * 64, 128)
    rng(RW + 1536, 128); rng(RW + 1664, 128)
    rng(3848 + 2 * hh * 128, 128); rng(3848 + (2 * hh + 1) * 128, 128)
    rng(4360 + 2 * hh * 64, 128); rng(4616 + 2 * hh * 64, 128)
    rng(4872 + 2 * hh * 128, 128); rng(4872 + (2 * hh + 1) * 128, 128)
    rng(5384 + 2 * hh * 128, 128); rng(5384 + (2 * hh + 1) * 128, 128)
    small = [-1] * 128
    for i in range(16):
        small[i] = 5896 + i
    small[32] = 2048 + 2 * hh; small[33] = 2048 + 2 * hh + 1
    small[64] = 2052 + 2 * hh; small[65] = 2052 + 2 * hh + 1
    cols.extend(small)
    assert len(cols) == NCOLB
    return np.array(cols)


def emit_norm_T(kb, x_d, g_sb, hnT, tg, xt, junk, ss, ident, pbs, tag, ntok_tiles=4, eps=EPS):
    for j in range(ntok_tiles):
        r0 = (tg * ntok_tiles + j) * 128
        kb.dma(xt[:, j, :], x_d[r0:r0 + 128, :], w=[("xt", j)])
        kb.act(junk[:, :], xt[:, j, :], AF.Square, r=[("xt", j)], w=["junk", ("ss", j)], accum_out=ss[:, j:j + 1])
        kb.ts(ss[:, j:j + 1], ss[:, j:j + 1], 1.0 / D, ALU.mult, eps, ALU.add, r=[("ss", j)], w=[("ss", j)])
        kb.act(ss[:, j:j + 1], ss[:, j:j + 1], AF.Sqrt, r=[("ss", j)], w=[("ss", j)])
        kb.recip(ss[:, j:j + 1], ss[:, j:j + 1], r=[("ss", j)], w=[("ss", j)])
        kb.ts(xt[:, j, :], xt[:, j, :], ss[:, j:j + 1], ALU.mult, r=[("xt", j), ("ss", j)], w=[("xt", j)], eng="gpsimd")
    for c in range(KC):
        pb, pbn = pbs[c % 2]
        for j in range(ntok_tiles):
            kb.tr(pb[:, j * 128:(j + 1) * 128], xt[:, j, c * 128:(c + 1) * 128], ident, r=[("xt", j)], w=[pbn])
        if c % 2 == 0:
            kb.ts(hnT[:, c, :], pb[:, 0:ntok_tiles * 128], g_sb[:, c:c + 1], ALU.mult, r=[pbn], w=[(tag, c)])
        else:
            kb.act(hnT[:, c, :], pb[:, 0:ntok_tiles * 128], AF.Identity, r=[pbn], w=[(tag, c)], scale=g_sb[:, c:c + 1])


def build_B(enable=("gdn", "rwkv", "pool", "gla"), debug=False):
    kb = KB()
    nc = kb.nc
    x_d = kb.din("x", [S, D])
    g1_d = kb.din("g1", [128, KC])
    w_d = kb.din("wsel", [D, NCOLB])
    cst_d = kb.din("cst", [128, CW])
    prm_d = kb.din("prm", [128, PRM_W])
    wsm_d = kb.din("wsm", [128, WSM_W])
    ysT_d = kb.dout("ysT", [1024, S])
    proj_d = kb.dout("projT", [NCOLB, S]) if debug else kb.dscr("projT", [NCOLB, S])

    cst = kb.sb("cst", [128, CW])
    prm = kb.sb("prm", [128, PRM_W])
    wsm = kb.sb("wsm", [128, WSM_W])
    g1 = kb.sb("g1", [128, KC])
    kb.dma(cst[:, :], cst_d, w=["cst"])
    kb.dma(prm[:, :], prm_d, w=["prm"])
    kb.dma(wsm[:, :], wsm_d, w=["wsm"])
    kb.dma(g1[:, :], g1_d, w=["g1"])
    ident = cst[:, C_ID:C_ID + 128]
    pbs = [(kb.ps("pb%d" % i), "pb%d" % i) for i in range(8)]

    kb.phase_begin()
    wsb = kb.sb("wsb", [128, KC, NCOLB], BF16, manual=True)
    wv = w_d.rearrange("(c p) n -> p c n", p=128)
    for c4 in range(4):
        kb.dma(wsb[:, c4 * 4:(c4 + 1) * 4, :], wv[:, c4 * 4:(c4 + 1) * 4, :], w=[("wsb", c4)], eng="gpsimd")
    xt = kb.sb("xt", [128, 4, D], manual=True)
    junk = kb.sb("junk", [128, D])
    ss = kb.sb("ss", [128, 4], manual=True)
    hnT = kb.sb("hnT", [128, KC, TB], BF16, manual=True)
    stg = kb.sb("stg", [128, 4, TB], manual=True)
    for tg in range(NB):
        emit_norm_T(kb, x_d, g1, hnT, tg, xt, junk, ss, ident, pbs[0:2], "hnT")
        for f in range(NT_B):
            pb, pbn = pbs[2 + f % 2]
            for c in range(KC):
                kb.mm(pb[:, :], wsb[:, c, f * 128:(f + 1) * 128], hnT[:, c, :], start=(c == 0), stop=(c == KC - 1),
                      r=[("wsb", c // 4), ("hnT", c)], w=[pbn])
            sl = f % 4
            if f % 2 == 0:
                kb.cp(stg[:, sl, :], pb[:, :], r=[pbn], w=[("stg", sl)])
            else:
                kb.cp(stg[:, sl, :], pb[:, :], r=[pbn], w=[("stg", sl)], eng="scalar")
            kb.dma(proj_d[f * 128:(f + 1) * 128, tg * TB:(tg + 1) * TB], stg[:, sl, :], r=[("stg", sl)], w=[("projd", f, tg)])

    kb.phase_end()
    ctx = dict(kb=kb, cst=cst, prm=prm, wsm=wsm, proj_d=proj_d, ysT_d=ysT_d, pbs=pbs, ident=ident)
    st = {}
    if "gla" in enable:
        st["gla"] = gla_setup(ctx)
    if "gdn" in enable:
        st["gdn"] = gdn_setup(ctx)
    if "rwkv" in enable:
        st["rwkv"] = rwkv_setup(ctx)
    if "pool" in enable:
        st["pool"] = pool_setup(ctx)
    for tb in range(NB):
        if "pool" in enable:
            pool_block(ctx, st["pool"], tb)
        if "gla" in enable:
            gla_block(ctx, st["gla"], tb)
        if "gdn" in enable:
            gdn_block(ctx, st["gdn"], tb)
        if "rwkv" in enable:
            rwkv_block(ctx, st["rwkv"], tb)
    return kb.finish()

PRM_W = 80
WSM_W = 896
P_GLA_FB, P_GLA_NG, P_GDN_NG, P_GDN_CONV, P_GDN_ALOG, P_GDN_DTB = 0, 1, 2, 3, 27, 29
P_RW, P_RW_MUWA, P_RW_MUGD, P_POOL_SC = 31, 51, 52, 53
W_FUP, W_WA, W_GUP, W_POOL = 0, 128, 384, 640


def make_prm(inp, L, hh):
    p = np.zeros((128, PRM_W), np.float32)
    g = lambda n: np.asarray(inp[n][L], np.float32)
    p[:, P_GLA_FB] = g("gla_f_bias")[2 * hh * 64:2 * hh * 64 + 128]
    p[:, P_GLA_NG] = g("gla_norm_g")
    p[:, P_GDN_NG] = g("gdn_norm_g")
    cw = g("gdn_conv_w")
    for hi, h in enumerate((2 * hh, 2 * hh + 1)):
        for t in range(3):
            for j in range(4):
                p[:, P_GDN_CONV + (hi * 3 + t) * 4 + j] = cw[j, t * 512 + h * 128:t * 512 + (h + 1) * 128]
        p[:, P_GDN_ALOG + hi] = g("gdn_a_log")[h]
        p[:, P_GDN_DTB + hi] = g("gdn_dt_bias")[h]
    mu = g("rwkv_mu")
    for pr in (0, 1):
        h0 = 4 * hh + 2 * pr
        sl = slice(h0 * 64, h0 * 64 + 128)
        b = P_RW + pr * 10
        p[:, b + 0] = mu[0:512][sl]; p[:, b + 1] = mu[512:1024][sl]; p[:, b + 2] = mu[1024:1536][sl]
        p[:, b + 3] = g("rwkv_w0")[sl]; p[:, b + 4] = g("rwkv_a0")[sl]
        p[:, b + 5] = g("rwkv_k_k")[sl]; p[:, b + 6] = g("rwkv_k_a")[sl]
        p[:, b + 7] = g("rwkv_r_k").reshape(-1)[sl]
        p[:, b + 8] = g("rwkv_ln_g")[sl]; p[:, b + 9] = g("rwkv_ln_b")[sl]
    p[:, P_RW_MUWA] = mu[1536:1664]
    p[:, P_RW_MUGD] = mu[1664:1792]
    for gi in (0, 1):
        gg = 2 * hh + gi
        p[:, P_POOL_SC + gi] = g("pool_scale")[gg * 128:(gg + 1) * 128]
        p[:, 55 + gi * 4 + gg] = 1.0 / (2 << gg)
        p[:, 55 + 8 + gi * 4 + gg] = 1.0
    return p


def make_wsm(inp, L, hh):
    w = np.zeros((128, WSM_W), np.float32)
    g = lambda n: np.asarray(inp[n][L], np.float32)
    w[0:16, W_FUP:W_FUP + 128] = g("gla_f_up")[:, 2 * hh * 64:2 * hh * 64 + 128]
    for pr in (0, 1):
        h0 = 4 * hh + 2 * pr
        sl = slice(h0 * 64, h0 * 64 + 128)
        w[0:64, W_WA + pr * 128:W_WA + (pr + 1) * 128] = g("rwkv_w_up")[:, sl]
        w[64:128, W_WA + pr * 128:W_WA + (pr + 1) * 128] = g("rwkv_a_up")[:, sl]
        w[:, W_GUP + pr * 128:W_GUP + (pr + 1) * 128] = g("rwkv_g_up")[:, sl]
    for gi in (0, 1):
        w[:, W_POOL + gi * 128:W_POOL + (gi + 1) * 128] = g("pool_w")[2 * hh + gi]
    return w


def v3(ap, c=64):
    return ap.rearrange("p (n c) -> p n c", c=c)


def emit_out_norm(ctx, name, o_sb, ssq, oT, ng_col, nch, pbank, dv=128):
    kb = ctx["kb"]; prm = ctx["prm"]; ident = ctx["ident"]
    pb, pbn = pbank
    kb.ts(ssq[0:64, 0:nch], ssq[0:64, 0:nch], 1.0 / dv, ALU.mult, EPS, ALU.add, r=[name + "ssq"], w=[name + "ssq"])
    kb.act(ssq[0:64, 0:nch], ssq[0:64, 0:nch], AF.Sqrt, r=[name + "ssq"], w=[name + "ssq"])
    kb.recip(ssq[0:64, 0:nch], ssq[0:64, 0:nch], r=[name + "ssq"], w=[name + "ssq"])
    for n in range(nch):
        sl = n % 4
        kb.ts(o_sb[0:64, n, :], o_sb[0:64, n, :], ssq[0:64, n:n + 1], ALU.mult, r=[(name + "o", n), name + "ssq"], w=[(name + "o", n)], eng="gpsimd")
        kb.tr(pb[0:dv, sl * 64:(sl + 1) * 64], o_sb[0:64, n, :], ident[0:64, 0:64], r=[(name + "o", n)], w=[(pbn, sl)])
        kb.ts(oT[0:dv, n * 64:(n + 1) * 64], pb[0:dv, sl * 64:(sl + 1) * 64], prm[0:dv, ng_col:ng_col + 1], ALU.mult, r=[(pbn, sl), "prm"], w=[name + "oT"])


def gla_setup(ctx):
    kb = ctx["kb"]
    st = {}
    for n in ("q", "k", "v0", "v1", "g0", "g1", "sp", "csp", "Ep", "Em", "Et"):
        st[n] = kb.sb("gla_" + n, [128, TB])
    st["oT0"] = st["sp"]
    st["oT1"] = st["q"]
    st["fd"] = kb.sb("gla_fd", [16, TB])
    st["cdec"] = kb.sb("gla_cdec", [128, 8])
    st["nfb"] = kb.sb("gla_nfb", [128, 1])
    st["S"] = kb.sb("gla_S", [128, 128], manual=True)
    st["attnT"] = kb.sb("gla_attnT", [64, 2, 64], manual=True)
    st["Vtm"] = kb.sb("gla_Vtm", [64, 2, 128], manual=True)
    st["Ktm"] = kb.sb("gla_Ktm", [64, 128])
    st["o"] = kb.sb("gla_o", [64, 16, 128], manual=True)
    st["ssq"] = kb.sb("gla_ssq", [64, 16])
    st["junk"] = kb.sb("gla_junk", [64, 128])
    kb.memset(st["S"][:, :], 0.0, w=["gla_S"])
    kb.ts(st["nfb"][:, :], ctx["prm"][:, P_GLA_FB:P_GLA_FB + 1], -1.0, ALU.mult, r=["prm"], w=["gla_nfb"])
    return st


def gla_block(ctx, st, tb):
    kb = ctx["kb"]; cst = ctx["cst"]; prm = ctx["prm"]; wsm = ctx["wsm"]; proj_d = ctx["proj_d"]
    pbs = ctx["pbs"]; ident = ctx["ident"]
    t0 = tb * TB
    for nm, f in (("q", 18), ("k", 19), ("v0", 20), ("v1", 21), ("g0", 22), ("g1", 23)):
        kb.dma(st[nm][:, :], proj_d[f * 128:(f + 1) * 128, t0:t0 + TB], r=[("projd", f, tb)], w=["gla_" + nm])
    kb.dma(st["fd"][:, :], proj_d[24 * 128:24 * 128 + 16, t0:t0 + TB], r=[("projd", 24, tb)], w=["gla_fd"])
    pb0, n0 = pbs[0]
    kb.mm(pb0[:, :], wsm[0:16, W_FUP:W_FUP + 128], st["fd"][0:16, :], r=["wsm", "gla_fd"], w=[n0])
    sp, csp, Ep, Em, Et = st["sp"], st["csp"], st["Ep"], st["Em"], st["Et"]
    kb.act(sp[:, :], pb0[:, :], AF.Exp, r=[n0, "gla_nfb"], w=["gla_sp"], scale=-1.0, bias=st["nfb"][:, 0:1])
    kb.act(sp[:, :], sp[:, :], AF.Ln, r=["gla_sp"], w=["gla_sp"], bias=1.0)
    kb.scan(csp[:, :], cst[:, C_RM:C_RM + TB], sp[:, :], 0.0, ALU.mult, ALU.add, r=["cst", "gla_sp"], w=["gla_csp"])
    kb.act(Ep[:, :], csp[:, :], AF.Exp, r=["gla_csp"], w=["gla_Ep"], scale=-1.0 / 16)
    kb.act(Em[:, :], csp[:, :], AF.Exp, r=["gla_csp"], w=["gla_Em"], scale=1.0 / 16)
    last = v3(csp[:, :])[:, :, 63:64]
    kb.tt(v3(Et[:, :]), v3(csp[:, :]), last.to_broadcast([128, 8, 64]), ALU.subtract, r=["gla_csp"], w=["gla_Et"])
    kb.act(Et[:, :], Et[:, :], AF.Exp, r=["gla_Et"], w=["gla_Et"], scale=1.0 / 16)
    kb.act(st["cdec"][:, :], csp[:, 63:TB:64], AF.Exp, r=["gla_csp"], w=["gla_cdec"], scale=-1.0 / 16)
    q, k = st["q"], st["k"]
    kb.stt(Ep[:, :], q[:, :], 0.125, Ep[:, :], ALU.mult, ALU.mult, r=["gla_q", "gla_Ep"], w=["gla_Ep"])
    kb.tt(Em[:, :], k[:, :], Em[:, :], ALU.mult, r=["gla_k", "gla_Em"], w=["gla_Em"])
    kb.tt(Et[:, :], k[:, :], Et[:, :], ALU.mult, r=["gla_k", "gla_Et"], w=["gla_Et"], eng="gpsimd")
    kb.act(st["g0"][:, :], st["g0"][:, :], AF.Silu, r=["gla_g0"], w=["gla_g0"])
    kb.act(st["g1"][:, :], st["g1"][:, :], AF.Silu, r=["gla_g1"], w=["gla_g1"])
    qdec, kdec, ktail = Ep, Em, Et
    S_ = st["S"]
    pb1, n1 = pbs[1]; pb2, n2 = pbs[2]; pb3, n3 = pbs[3]; pb4, n4 = pbs[4]
    mUI = cst[0:64, C_UI:C_UI + 64]
    for n in range(8):
        c0 = n * 64
        po = 0
        par = n % 2
        pb0, n0 = pbs[4 * par]; pb1, n1 = pbs[4 * par + 1]; pb2, n2 = pbs[4 * par + 2]; pb3, n3 = pbs[4 * par + 3]
        for h in (0, 1):
            hs = slice(64 * h, 64 * h + 64)
            kb.mm(pb0[0:64, po + 64 * h:po + 64 * h + 64], kdec[hs, c0:c0 + 64], qdec[hs, c0:c0 + 64],
                  r=["gla_Em", "gla_Ep"], w=[(n0, "a", par, h)])
            kb.tt(st["attnT"][0:64, h, :], pb0[0:64, po + 64 * h:po + 64 * h + 64], mUI, ALU.mult,
                  r=[(n0, "a", par, h), "cst"], w=[("gla_attnT", h)])
            vt = st["v%d" % h]
            kb.tr(pb1[0:64, po + 128 * h:po + 128 * h + 128], vt[:, c0:c0 + 64], ident, r=["gla_v%d" % h], w=[(n1, par, h)])
            kb.cp(st["Vtm"][0:64, h, :], pb1[0:64, po + 128 * h:po + 128 * h + 128], r=[(n1, par, h)], w=[("gla_Vtm", h)], eng="scalar")
        kb.tr(pb0[0:64, po + 128:po + 256], ktail[:, c0:c0 + 64], ident, r=["gla_Et"], w=[(n0, "k", par)])
        kb.cp(st["Ktm"][0:64, :], pb0[0:64, po + 128:po + 256], r=[(n0, "k", par)], w=["gla_Ktm"])
        for h in (0, 1):
            hs = slice(64 * h, 64 * h + 64)
            o_ps = pb2[0:64, po + 128 * h:po + 128 * h + 128]
            o_ps2 = pb2[0:64, 256 + 128 * h:256 + 128 * h + 128]
            kb.mm(o_ps, qdec[hs, c0:c0 + 64], S_[hs, :], r=["gla_Ep", ("gla_S", h)], w=[(n2, par, h)])
            kb.mm(o_ps2, st["attnT"][0:64, h, :], st["Vtm"][0:64, h, :],
                  r=[("gla_attnT", h), ("gla_Vtm", h)], w=[(n2, par, h)])
        for h in (0, 1):
            hs = slice(64 * h, 64 * h + 64)
            sn = pb3[:, po + 128 * h:po + 128 * h + 128]
            kb.mm(sn, st["Ktm"][0:64, :], st["Vtm"][0:64, h, :], r=["gla_Ktm", ("gla_Vtm", h)], w=[(n3, par, h)])
            kb.stt(S_[hs, :], S_[hs, :], st["cdec"][hs, n:n + 1], pb3[hs, po + 128 * h:po + 128 * h + 128], ALU.mult, ALU.add,
                   r=[("gla_S", h), "gla_cdec", (n3, par, h)], w=[("gla_S", h)])
            o_ps = pb2[0:64, po + 128 * h:po + 128 * h + 128]
            o_ps2 = pb2[0:64, 256 + 128 * h:256 + 128 * h + 128]
            osl = st["o"][0:64, n * 2 + h, :]
            kb.act(osl, o_ps, AF.Identity, r=[(n2, par, h)], w=[("gla_o", n * 2 + h)])
            kb.tt(osl, osl, o_ps2, ALU.add, r=[("gla_o", n * 2 + h)], w=[("gla_o", n * 2 + h)])
            kb.act(st["junk"][0:64, :], osl, AF.Square, r=[("gla_o", n * 2 + h)], w=["gla_junk", "gla_ssq"],
                   accum_out=st["ssq"][0:64, n * 2 + h:n * 2 + h + 1])
    emit_out_norm2(ctx, "gla_", st["o"], st["ssq"], (st["oT0"], st["oT1"]), P_GLA_NG, pbs[0], pbs[4])
    for h in (0, 1):
        oT = st["oT%d" % h]
        kb.tt(oT[:, :], oT[:, :], st["g%d" % h][:, :], ALU.mult, r=["gla_oT%d" % h, "gla_g%d" % h], w=["gla_oT%d" % h])
        kb.dma(ctx["ysT_d"][768 + h * 128:768 + (h + 1) * 128, t0:t0 + TB], oT[:, :], r=["gla_oT%d" % h], w=["ysd"])


def emit_out_norm2(ctx, name, o_sb, ssq, oTs, ng_col, pbankA, pbankB, dv=128):
    kb = ctx["kb"]; prm = ctx["prm"]; ident = ctx["ident"]
    kb.ts(ssq[0:64, :], ssq[0:64, :], 1.0 / dv, ALU.mult, EPS, ALU.add, r=[name + "ssq"], w=[name + "ssq"])
    kb.act(ssq[0:64, :], ssq[0:64, :], AF.Sqrt, r=[name + "ssq"], w=[name + "ssq"])
    kb.recip(ssq[0:64, :], ssq[0:64, :], r=[name + "ssq"], w=[name + "ssq"])
    for i in range(16):
        n, h = i // 2, i % 2
        sl = i % 8
        pb, pbn = pbankA if i % 2 == 0 else pbankB
        kb.ts(o_sb[0:64, i, :], o_sb[0:64, i, :], ssq[0:64, i:i + 1], ALU.mult, r=[(name + "o", i), name + "ssq"], w=[(name + "o", i)], eng="gpsimd")
        kb.tr(pb[0:dv, sl * 64:(sl + 1) * 64], o_sb[0:64, i, :], ident[0:64, 0:64], r=[(name + "o", i)], w=[(pbn, sl)])
        kb.ts(oTs[h][0:dv, n * 64:(n + 1) * 64], pb[0:dv, sl * 64:(sl + 1) * 64], prm[0:dv, ng_col:ng_col + 1], ALU.mult,
              r=[(pbn, sl), "prm"], w=[name + "oT%d" % h])


def emit_inverse_multi(kb, units):
    for u in units:
        kb.tt(u["Q"], u["N"], u["ident"], ALU.add)
    for lvl in range(5):
        last = (lvl == 4)
        for u in units:
            M, N = (u["M"], u["N"]) if lvl % 2 == 0 else (u["M2"], u["N2"])
            Mn, Nn = (u["M2"], u["N2"]) if lvl % 2 == 0 else (u["M"], u["N"])
            pM = u["pbM"][0][0:64, 0:64]
            pN = u["pbN"][0][0:64, 0:64]
            kb.mm(pM, N, M)
            if not last:
                kb.mm(pN, M, N)
            kb.cp(Mn, pM, eng="scalar")
            if not last:
                kb.cp(Nn, pN)
        for u in units:
            Mn = u["M2"] if lvl % 2 == 0 else u["M"]
            pQ = u["pbM"][0][0:64, 64:128]
            kb.mm(pQ, Mn, u["Q"])
            kb.tt(u["Q"], u["Q"], pQ, ALU.add)


P_POOL_SEL = 55


def pool_setup(ctx):
    kb = ctx["kb"]
    st = {}
    st["u"] = kb.sb("pl_u", [128, HALO + TB])
    st["sa"] = kb.sb("pl_sa", [128, HALO + TB])
    st["sb"] = kb.sb("pl_sb", [128, HALO + TB])
    st["acc"] = kb.sb("pl_acc", [128, TB])
    st["tmp"] = kb.sb("pl_tmp", [128, 16])
    st["y"] = kb.sb("pl_y", [128, TB])
    return st


def pool_block(ctx, st, tb):
    kb = ctx["kb"]; cst = ctx["cst"]; prm = ctx["prm"]; wsm = ctx["wsm"]; proj_d = ctx["proj_d"]; pbs = ctx["pbs"]
    t0 = tb * TB
    W = HALO + TB
    for gi in (0, 1):
        f = 16 + gi
        u = st["u"]
        if tb == 0:
            kb.memset(u[:, 0:HALO], 0.0)
            kb.dma(u[:, HALO:], proj_d[f * 128:(f + 1) * 128, 0:TB], r=[("projd", f, 0)])
        else:
            kb.dma(u[:, :], proj_d[f * 128:(f + 1) * 128, t0 - HALO:t0 + TB], r=[("projd", f, tb), ("projd", f, tb - 1)])
        acc = st["acc"]; tmp = st["tmp"]
        kb.ts(acc[:, :], u[:, HALO:], -1.0, ALU.mult)
        prev = u
        for k in range(4):
            sh = 1 << k
            s_ = st["sa"] if k % 2 == 0 else st["sb"]
            kb.tt(s_[:, sh:W], prev[:, sh:W], prev[:, 0:W - sh], ALU.add, eng=("gpsimd" if k % 2 else "vector"))
            kb.cp(s_[:, 0:sh], prev[:, 0:sh], eng="gpsimd")
            c = P_POOL_SEL + gi * 4 + k
            if tb == 0:
                c2 = P_POOL_SEL + 8 + gi * 4 + k
                kb.stt(acc[:, 16:], s_[:, HALO + 16:], prm[:, c:c + 1], acc[:, 16:], ALU.mult, ALU.add)
                kb.tt(tmp[:, :], s_[:, HALO:HALO + 16], cst[:, C_PC + k * 16:C_PC + (k + 1) * 16], ALU.mult)
                kb.stt(acc[:, 0:16], tmp[:, :], prm[:, c2:c2 + 1], acc[:, 0:16], ALU.mult, ALU.add)
            else:
                kb.stt(acc[:, :], s_[:, HALO:], prm[:, c:c + 1], acc[:, :], ALU.mult, ALU.add)
            prev = s_
        pb, pbn = pbs[gi]
        kb.mm(pb[:, :], wsm[:, W_POOL + gi * 128:W_POOL + (gi + 1) * 128], acc[:, :])
        y = st["y"]
        kb.ts(y[:, :], pb[:, :], prm[:, P_POOL_SC + gi:P_POOL_SC + gi + 1], ALU.mult)
        kb.dma(ctx["ysT_d"][512 + gi * 128:512 + (gi + 1) * 128, t0:t0 + TB], y[:, :], w=["ysd"])


def gdn_setup(ctx):
    kb = ctx["kb"]; prm = ctx["prm"]
    st = {}
    for hi in (0, 1):
        p = "gd%d_" % hi
        d = {}
        d["raw"] = kb.sb(p + "raw", [128, HALO + TB])
        for n in ("q", "k", "v"):
            d[n + "raw"] = d["raw"]
            d[n] = kb.sb(p + n, [128, TB])
        for n in ("z", "sq", "bl", "al", "csg", "egc", "oT"):
            d[n] = kb.sb(p + n, [128, TB])
        for n in ("Dm", "E2", "DTb"):
            d[n] = kb.sb(p + n, [64, TB])
        d["tmp3"] = d["sq"]
        d["D1"] = d["al"]
        d["DTi"] = d["E2"]
        for n in ("btok", "nbtok", "ctok", "egtok", "begtok", "egltok"):
            d[n] = kb.sb(p + n, [64, 8])
        d["cd"] = kb.sb(p + "cd", [128, 8])
        d["eal"] = kb.sb(p + "eal", [128, 1])
        d["S"] = kb.sb(p + "S", [128, 128])
        for n in ("M", "N", "Q", "M2", "N2", "attnT"):
            d[n] = kb.sb(p + n, [64, 64])
        for n in ("kbg", "ktl", "vb", "u", "vnew"):
            d[n] = kb.sb(p + n, [64, 128])
        d["wT"] = kb.sb(p + "wT", [128, 64])
        d["o"] = kb.sb(p + "o", [64, 8, 128], manual=True)
        d["ssq"] = kb.sb(p + "ssq", [64, 8])
        d["junk"] = kb.sb(p + "junk", [64, 128])
        kb.memset(d["S"][:, :], 0.0)
        kb.act(d["eal"][:, :], prm[:, P_GDN_ALOG + hi:P_GDN_ALOG + hi + 1], AF.Exp)
        st[hi] = d
    return st


def gdn_block(ctx, st, tb):
    kb = ctx["kb"]; cst = ctx["cst"]; prm = ctx["prm"]; proj_d = ctx["proj_d"]; pbs = ctx["pbs"]; ident = ctx["ident"]
    t0 = tb * TB
    i64 = cst[0:64, C_ID:C_ID + 64]
    mUI = cst[0:64, C_UI:C_UI + 64]; mUS = cst[0:64, C_US:C_US + 64]; mLS = cst[0:64, C_LS:C_LS + 64]
    ones = cst[:, C_ONE:C_ONE + 128]
    bc3 = lambda m: m.unsqueeze(1).to_broadcast([64, 8, 64])
    for hi in (0, 1):
        d = st[hi]
        bk = pbs[4 * hi:4 * hi + 4]
        for ti, n in enumerate(("q", "k", "v")):
            f = hi * 4 + ti
            raw = d[n + "raw"]
            if tb == 0:
                kb.memset(raw[:, 0:HALO], 0.0)
                kb.dma(raw[:, HALO:], proj_d[f * 128:(f + 1) * 128, 0:TB], r=[("projd", f, 0)])
            else:
                kb.dma(raw[:, :], proj_d[f * 128:(f + 1) * 128, t0 - HALO:t0 + TB], r=[("projd", f, tb), ("projd", f, tb - 1)])
            x = d[n]
            cb = P_GDN_CONV + (hi * 3 + ti) * 4
            kb.ts(x[:, :], raw[:, HALO - 3:HALO - 3 + TB], prm[:, cb:cb + 1], ALU.mult)
            for j in (1, 2, 3):
                kb.stt(x[:, :], raw[:, HALO - 3 + j:HALO - 3 + j + TB], prm[:, cb + j:cb + j + 1], x[:, :], ALU.mult, ALU.add)
            kb.act(x[:, :], x[:, :], AF.Silu)
        f = hi * 4 + 3
        kb.dma(d["z"][:, :], proj_d[f * 128:(f + 1) * 128, t0:t0 + TB], r=[("projd", f, tb)])
        kb.act(d["z"][:, :], d["z"][:, :], AF.Silu)
        rb = 24 * 128 + 32 + hi
        kb.dma(d["bl"][:, :], proj_d[rb:rb + 1, t0:t0 + TB].broadcast_to([128, TB]), r=[("projd", 24, tb)])
        ra = 24 * 128 + 64 + hi
        kb.dma(d["al"][:, :], proj_d[ra:ra + 1, t0:t0 + TB].broadcast_to([128, TB]), r=[("projd", 24, tb)])
        for n, sc in (("q", 128.0 ** -0.5), ("k", 1.0)):
            x = d[n]
            kb.tt(d["sq"][:, :], x[:, :], x[:, :], ALU.mult, eng="gpsimd")
            pb, pbn = bk[0]
            kb.mm(pb[:, :], ones, d["sq"][:, :])
            kb.act(d["sq"][:, :], pb[:, :], AF.Sqrt, bias=1e-6)
            kb.recip(d["sq"][:, :], d["sq"][:, :])
            kb.stt(x[:, :], x[:, :], sc, d["sq"][:, :], ALU.mult, ALU.mult)
        bl, al, csg = d["bl"], d["al"], d["csg"]
        kb.act(bl[:, :], bl[:, :], AF.Sigmoid)
        kb.act(al[:, :], al[:, :], AF.Exp, bias=prm[:, P_GDN_DTB + hi:P_GDN_DTB + hi + 1])
        kb.act(al[:, :], al[:, :], AF.Ln, bias=1.0)
        kb.ts(al[:, :], al[:, :], d["eal"][:, 0:1], ALU.mult)
        kb.scan(csg[:, :], cst[:, C_RM:C_RM + TB], al[:, :], 0.0, ALU.mult, ALU.add)
        kb.act(d["egc"][:, :], csg[:, :], AF.Exp, scale=-1.0)
        kb.act(d["cd"][:, :], csg[:, 63:TB:64], AF.Exp, scale=-1.0)
        t3 = v3(d["tmp3"][0:64, :])
        kb.tt(t3, v3(bl[0:64, :]), bc3(i64), ALU.mult)
        kb.P.op("vector", lambda e, o=d["btok"], i=t3: e.tensor_reduce(out=o[:, :], in_=i, axis=AX.X, op=ALU.add),
                reads=kb.ak([d["tmp3"][:, :]], ()), writes=kb.ak([d["btok"][:, :]], ()))
        kb.tt(t3, v3(csg[0:64, :]), bc3(i64), ALU.mult)
        kb.P.op("vector", lambda e, o=d["ctok"], i=t3: e.tensor_reduce(out=o[:, :], in_=i, axis=AX.X, op=ALU.add),
                reads=kb.ak([d["tmp3"][:, :]], ()), writes=kb.ak([d["ctok"][:, :]], ()))
        kb.ts(d["nbtok"][:, :], d["btok"][:, :], -1.0, ALU.mult)
        kb.act(d["egtok"][:, :], d["ctok"][:, :], AF.Exp, scale=-1.0)
        kb.tt(d["begtok"][:, :], d["egtok"][:, :], d["btok"][:, :], ALU.mult)
        kb.tt(d["egltok"][:, :], d["ctok"][:, :], csg[0:64, 63:TB:64], ALU.subtract)
        kb.act(d["egltok"][:, :], d["egltok"][:, :], AF.Exp)
        D1 = v3(d["D1"][0:64, :])
        kb.tt(D1, v3(csg[0:64, :]), d["ctok"][:, :].unsqueeze(2).to_broadcast([64, 8, 64]), ALU.subtract)
        kb.ts(d["Dm"][:, :], d["D1"][0:64, :], 0.0, ALU.min)
        kb.act(d["Dm"][:, :], d["Dm"][:, :], AF.Exp)
        kb.tt(v3(d["Dm"][:, :]), v3(d["Dm"][:, :]), bc3(mLS), ALU.mult, eng="gpsimd")
        kb.ts(d["E2"][:, :], d["D1"][0:64, :], -1.0, ALU.mult, 0.0, ALU.min)
        kb.act(d["E2"][:, :], d["E2"][:, :], AF.Exp)
        kb.tt(v3(d["DTb"][:, :]), v3(d["E2"][0:64, :]), bc3(mUS), ALU.mult, eng="gpsimd")
        kb.stt(d["DTb"][:, :], d["DTb"][:, :], -1.0, bl[0:64, :], ALU.mult, ALU.mult)
        kb.tt(v3(d["DTi"][0:64, :]), v3(d["E2"][0:64, :]), bc3(mUI), ALU.mult, eng="gpsimd")
        kb.tt(d["egc"][:, :], d["q"][:, :], d["egc"][:, :], ALU.mult)
    import os as _os
    STG = int(_os.environ.get("GDN_STAGE", "9"))
    for n in range(8 if STG >= 1 else 0):
        c0 = n * 64
        cs = slice(c0, c0 + 64)
        units = []
        for hi in (0, 1):
            d = st[hi]
            bk = pbs[4 * hi:4 * hi + 4]
            kT = d["k"]; qT = d["q"]; vT = d["v"]
            pA = bk[0][0]
            SUB = _os.environ.get("GDN_SUB", "ab")
            if "a" in SUB:
                kb.mm(pA[0:64, 0:64], kT[:, cs], kT[:, cs])
                kb.mm(pA[0:64, 64:128], kT[:, cs], qT[:, cs])
                kb.stt(d["M"][:, :], pA[0:64, 0:64], d["nbtok"][:, n:n + 1], d["Dm"][:, cs], ALU.mult, ALU.mult)
                kb.tt(d["N"][:, :], pA[0:64, 0:64], d["DTb"][:, cs], ALU.mult)
                kb.tt(d["attnT"][:, :], pA[0:64, 64:128], d["DTi"][0:64, cs], ALU.mult)
            pT = bk[1][0]
            if "b" in SUB:
                kb.tr(pT[0:64, 0:128], kT[:, cs], ident)
                kb.tr(pT[0:64, 128:256], vT[:, cs], ident)
                kb.ts(d["kbg"][:, :], pT[0:64, 0:128], d["begtok"][:, n:n + 1], ALU.mult)
                kb.act(d["ktl"][:, :], pT[0:64, 0:128], AF.Identity, scale=d["egltok"][:, n:n + 1])
                kb.ts(d["vb"][:, :], pT[0:64, 128:256], d["btok"][:, n:n + 1], ALU.mult)
            units.append(dict(M=d["M"][:, :], N=d["N"][:, :], Q=d["Q"][:, :], M2=d["M2"][:, :], N2=d["N2"][:, :],
                              pbM=bk[2], pbN=bk[3], ident=i64))
        if STG < 2:
            continue
        emit_inverse_multi(kb, units)
        if STG < 3:
            continue
        for hi in (0, 1):
            d = st[hi]
            bk = pbs[4 * hi:4 * hi + 4]
            pU = bk[1][0]
            kb.mm(pU[0:64, 0:128], d["Q"][:, :], d["vb"][:, :])
            kb.mm(pU[0:128, 128:192], d["kbg"][:, :], d["Q"][:, :])
            kb.cp(d["u"][:, :], pU[0:64, 0:128], eng="scalar")
            kb.cp(d["wT"][:, :], pU[0:128, 128:192])
        if STG < 4:
            continue
        for hi in (0, 1):
            d = st[hi]
            bk = pbs[4 * hi:4 * hi + 4]
            pV = bk[0][0]
            kb.mm(pV[0:64, 0:128], d["wT"][:, :], d["S"][:, :])
            kb.tt(d["vnew"][:, :], d["u"][:, :], pV[0:64, 0:128], ALU.subtract)
            pO = bk[2][0]
            kb.mm(pO[0:64, 0:128], d["egc"][:, cs], d["S"][:, :], start=True, stop=False)
            kb.mm(pO[0:64, 0:128], d["attnT"][:, :], d["vnew"][:, :], start=False, stop=True)
            pS = bk[3][0]
            kb.mm(pS[:, 0:128], d["ktl"][:, :], d["vnew"][:, :])
            kb.stt(d["S"][:, :], d["S"][:, :], d["cd"][:, n:n + 1], pS[:, 0:128], ALU.mult, ALU.add)
            kb.act(d["o"][0:64, n, :], pO[0:64, 0:128], AF.Identity, w=[("gd%d_o" % hi, n)])
            kb.act(d["junk"][:, :], pO[0:64, 0:128], AF.Square, accum_out=d["ssq"][:, n:n + 1])
    for hi in (0, 1):
        d = st[hi]
        emit_out_norm(ctx, "gd%d_" % hi, d["o"], d["ssq"], d["oT"], P_GDN_NG, 8, pbs[4 * hi + 1])
        kb.tt(d["oT"][:, :], d["oT"][:, :], d["z"][:, :], ALU.mult)
        kb.dma(ctx["ysT_d"][hi * 128:(hi + 1) * 128, t0:t0 + TB], d["oT"][:, :], w=["ysd"])


RW_DECAY = 0.606531
RW_LN_EPS = 64e-5


def rwkv_setup(ctx):
    kb = ctx["kb"]
    st = {}
    st["raw"] = kb.sb("rw_raw", [128, HALO + TB])
    for n in ("waraw", "gdraw", "rraw", "kraw", "vraw"):
        st[n] = st["raw"]
    for n in ("was", "gds", "r", "k", "v", "pw", "cs", "alr", "gate", "kk", "k2", "kka", "Ep", "Em", "Epv", "Etl",
              "bonus", "tmp"):
        st[n] = kb.sb("rw_" + n, [128, TB])
    st["At"] = st["alr"]
    st["Ktl"] = st["k"]
    st["yT"] = st["pw"]
    st["GamC"] = kb.sb("rw_GamC", [128, 8])
    for p in (0, 1):
        st["H%d" % p] = kb.sb("rw_H%d" % p, [128, 64])
        kb.memset(st["H%d" % p][:, :], 0.0)
    for h in (0, 1):
        for n in ("M", "N", "Q", "M2", "N2", "AbkT", "AraT", "ArkT"):
            st[n + str(h)] = kb.sb("rw_%s%d" % (n, h), [64, 64])
    for n in ("Vtm", "Ktm", "Atm", "Xsb", "Usb"):
        st[n] = kb.sb("rw_" + n, [64, 128])
    st["Ysb"] = kb.sb("rw_Ysb", [64, 8, 128])
    st["s1"] = kb.sb("rw_s1", [64, 16])
    st["s2"] = kb.sb("rw_s2", [64, 16])
    st["sm"] = kb.sb("rw_sm", [64, 16])
    st["junk"] = kb.sb("rw_junk", [64, 64])
    return st


def rwkv_block(ctx, st, tb):
    kb = ctx["kb"]; cst = ctx["cst"]; prm = ctx["prm"]; wsm = ctx["wsm"]; proj_d = ctx["proj_d"]; pbs = ctx["pbs"]; ident = ctx["ident"]
    t0 = tb * TB
    i64 = cst[0:64, C_ID:C_ID + 64]
    mUI = cst[0:64, C_UI:C_UI + 64]; mUS = cst[0:64, C_US:C_US + 64]; mLS = cst[0:64, C_LS:C_LS + 64]
    bo = cst[:, C_BO:C_BO + 128]
    tmp = st["tmp"]

    def load_shift(rawn, f, outn, mucol):
        raw = st[rawn]
        if tb == 0:
            kb.memset(raw[:, 0:HALO], 0.0)
            kb.dma(raw[:, HALO:], proj_d[f * 128:(f + 1) * 128, 0:TB], r=[("projd", f, 0)])
        else:
            kb.dma(raw[:, :], proj_d[f * 128:(f + 1) * 128, t0 - HALO:t0 + TB], r=[("projd", f, tb), ("projd", f, tb - 1)])
        kb.tt(tmp[:, :], raw[:, HALO - 1:HALO - 1 + TB], raw[:, HALO:HALO + TB], ALU.subtract, eng="gpsimd")
        kb.stt(st[outn][:, :], tmp[:, :], prm[:, mucol:mucol + 1], raw[:, HALO:HALO + TB], ALU.mult, ALU.add)

    load_shift("waraw", 14, "was", P_RW_MUWA)
    load_shift("gdraw", 15, "gds", P_RW_MUGD)
    kb.act(st["was"][0:64, :], st["was"][0:64, :], AF.Tanh)
    kb.act(st["gds"][:, :], st["gds"][:, :], AF.Sigmoid)
    for p in (0, 1):
        b = P_RW + p * 10
        col = lambda j: prm[:, b + j:b + j + 1]
        load_shift("rraw", 8 + 3 * p + 0, "r", b + 0)
        load_shift("kraw", 8 + 3 * p + 1, "k", b + 1)
        load_shift("vraw", 8 + 3 * p + 2, "v", b + 2)
        r, k, v, pw, cs, alr = st["r"], st["k"], st["v"], st["pw"], st["cs"], st["alr"]
        kk, k2, kka, Ep, Em, Epv, Etl, At, Ktl = st["kk"], st["k2"], st["kka"], st["Ep"], st["Em"], st["Epv"], st["Etl"], st["At"], st["Ktl"]
        H = st["H%d" % p]
        pb0 = pbs[0][0]; pb1 = pbs[1][0]; pb2 = pbs[2][0]
        kb.mm(pb0[:, :], wsm[0:64, W_WA + p * 128:W_WA + (p + 1) * 128], st["was"][0:64, :])
        kb.act(pw[:, :], pb0[:, :], AF.Sigmoid, bias=col(3))
        kb.ts(pw[:, :], pw[:, :], RW_DECAY, ALU.mult)
        kb.scan(cs[:, :], cst[:, C_RM:C_RM + TB], pw[:, :], 0.0, ALU.mult, ALU.add)
        kb.mm(pb1[:, :], wsm[64:128, W_WA + p * 128:W_WA + (p + 1) * 128], st["was"][64:128, :])
        kb.act(alr[:, :], pb1[:, :], AF.Sigmoid, bias=col(4))
        kb.mm(pb2[:, :], wsm[:, W_GUP + p * 128:W_GUP + (p + 1) * 128], st["gds"][:, :])
        kb.cp(st["gate"][:, :], pb2[:, :], eng="scalar")
        kb.ts(kk[:, :], k[:, :], col(5), ALU.mult)
        kb.tt(tmp[:, :], kk[:, :], kk[:, :], ALU.mult, eng="gpsimd")
        kb.mm(pb0[:, :], bo, tmp[:, :])
        kb.act(tmp[:, :], pb0[:, :], AF.Sqrt, bias=1e-6)
        kb.recip(tmp[:, :], tmp[:, :])
        kb.tt(kk[:, :], kk[:, :], tmp[:, :], ALU.mult)
        kb.ts(tmp[:, :], alr[:, :], -1.0, ALU.add, col(6), ALU.mult)
        kb.stt(k2[:, :], tmp[:, :], 1.0, k[:, :], ALU.add, ALU.mult)
        kb.tt(kka[:, :], kk[:, :], alr[:, :], ALU.mult, eng="gpsimd")
        kb.act(Ep[:, :], cs[:, :], AF.Exp, scale=-1.0)
        kb.act(Em[:, :], cs[:, :], AF.Exp)
        kb.tt(tmp[:, :], cs[:, :], pw[:, :], ALU.subtract)
        kb.act(Epv[:, :], tmp[:, :], AF.Exp, scale=-1.0)
        last = v3(cs[:, :])[:, :, 63:64]
        kb.tt(v3(Etl[:, :]), v3(cs[:, :]), last.to_broadcast([128, 8, 64]), ALU.subtract)
        kb.act(Etl[:, :], Etl[:, :], AF.Exp)
        kb.act(st["GamC"][:, :], cs[:, 63:TB:64], AF.Exp, scale=-1.0)
        kb.stt(Epv[:, :], kk[:, :], -1.0, Epv[:, :], ALU.mult, ALU.mult)
        kb.tt(At[:, :], kka[:, :], Em[:, :], ALU.mult)
        kb.tt(Em[:, :], k2[:, :], Em[:, :], ALU.mult, eng="gpsimd")
        kb.tt(Ep[:, :], r[:, :], Ep[:, :], ALU.mult)
        kb.tt(Ktl[:, :], k2[:, :], Etl[:, :], ALU.mult, eng="gpsimd")
        kb.tt(Etl[:, :], kka[:, :], Etl[:, :], ALU.mult)
        Bt, Kt, Rt, Atl = Epv, Em, Ep, Etl
        kb.tt(tmp[:, :], r[:, :], k2[:, :], ALU.mult, eng="gpsimd")
        kb.ts(tmp[:, :], tmp[:, :], col(7), ALU.mult)
        kb.mm(pb0[:, :], bo, tmp[:, :])
        kb.tt(st["bonus"][:, :], pb0[:, :], v[:, :], ALU.mult)
        for n in range(8):
            c0 = n * 64
            cs_ = slice(c0, c0 + 64)
            units = []
            for h in (0, 1):
                hs = slice(64 * h, 64 * h + 64)
                pS = pbs[4 * h][0]
                kb.mm(pS[0:64, 0:64], Bt[hs, cs_], At[hs, cs_])
                kb.mm(pS[0:64, 64:128], At[hs, cs_], Bt[hs, cs_])
                kb.mm(pS[0:64, 128:192], Kt[hs, cs_], Bt[hs, cs_])
                kb.mm(pS[0:64, 192:256], At[hs, cs_], Rt[hs, cs_])
                kb.mm(pS[0:64, 256:320], Kt[hs, cs_], Rt[hs, cs_])
                kb.tt(st["M%d" % h][:, :], pS[0:64, 0:64], mLS, ALU.mult)
                kb.tt(st["N%d" % h][:, :], pS[0:64, 64:128], mUS, ALU.mult)
                kb.tt(st["AbkT%d" % h][:, :], pS[0:64, 128:192], mUS, ALU.mult)
                kb.tt(st["AraT%d" % h][:, :], pS[0:64, 192:256], mUI, ALU.mult)
                kb.tt(st["ArkT%d" % h][:, :], pS[0:64, 256:320], mUI, ALU.mult)
                units.append(dict(M=st["M%d" % h][:, :], N=st["N%d" % h][:, :], Q=st["Q%d" % h][:, :], M2=st["M2%d" % h][:, :],
                                  N2=st["N2%d" % h][:, :], pbM=pbs[4 * h + 2], pbN=pbs[4 * h + 3], ident=i64))
            pT = pbs[1][0]
            kb.tr(pT[0:64, 0:128], v[:, cs_], ident)
            kb.tr(pT[0:64, 128:256], Ktl[:, cs_], ident)
            kb.tr(pT[0:64, 256:384], Atl[:, cs_], ident)
            kb.cp(st["Vtm"][:, :], pT[0:64, 0:128], eng="scalar")
            kb.cp(st["Ktm"][:, :], pT[0:64, 128:256], eng="scalar")
            kb.cp(st["Atm"][:, :], pT[0:64, 256:384], eng="scalar")
            emit_inverse_multi(kb, units)
            for h in (0, 1):
                hs = slice(64 * h, 64 * h + 64)
                hc = slice(64 * h, 64 * h + 64)
                pX = pbs[4 * h + 1][0]
                kb.mm(pX[0:64, 0:64], Bt[hs, cs_], H[hs, :])
                kb.mm(pX[0:64, 256:320], st["AbkT%d" % h][:, :], st["Vtm"][:, hc])
                kb.cp(st["Xsb"][:, hc], pX[0:64, 0:64], eng="scalar")
                kb.tt(st["Xsb"][:, hc], st["Xsb"][:, hc], pX[0:64, 256:320], ALU.add)
                kb.mm(pX[0:64, 64:128], st["Q%d" % h][:, :], st["Xsb"][:, hc])
                kb.cp(st["Usb"][:, hc], pX[0:64, 64:128])
                kb.mm(pX[0:64, 128:192], Rt[hs, cs_], H[hs, :])
                kb.mm(pX[0:64, 320:384], st["AraT%d" % h][:, :], st["Usb"][:, hc], start=True, stop=False)
                kb.mm(pX[0:64, 320:384], st["ArkT%d" % h][:, :], st["Vtm"][:, hc], start=False, stop=True)
                kb.act(st["Ysb"][:, n, hc], pX[0:64, 128:192], AF.Identity)
                kb.stt(st["Ysb"][:, n, hc], pX[0:64, 320:384], 1.0, st["Ysb"][:, n, hc], ALU.mult, ALU.add,
                       accum_out=st["s1"][:, n * 2 + h:n * 2 + h + 1])
                kb.act(st["junk"][:, :], st["Ysb"][:, n, hc], AF.Square, accum_out=st["s2"][:, n * 2 + h:n * 2 + h + 1])
            pH = pbs[0][0]
            kb.mm(pH[:, 0:128], st["Atm"][:, :], st["Usb"][:, :], start=True, stop=False)
            kb.mm(pH[:, 0:128], st["Ktm"][:, :], st["Vtm"][:, :], start=False, stop=True)
            for h in (0, 1):
                hs = slice(64 * h, 64 * h + 64)
                kb.stt(H[hs, :], H[hs, :], st["GamC"][hs, n:n + 1], pH[hs, 64 * h:64 * h + 64], ALU.mult, ALU.add)
        s1, s2, sm = st["s1"], st["s2"], st["sm"]
        kb.ts(s1[:, :], s1[:, :], 1.0 / 64, ALU.mult)
        kb.tt(sm[:, :], s1[:, :], s1[:, :], ALU.mult)
        kb.stt(s2[:, :], s2[:, :], 1.0 / 64, sm[:, :], ALU.mult, ALU.subtract)
        kb.ts(s2[:, :], s2[:, :], RW_LN_EPS, ALU.add)
        kb.act(s2[:, :], s2[:, :], AF.Sqrt)
        kb.recip(s2[:, :], s2[:, :])
        for n in range(8):
            for h in (0, 1):
                hc = slice(64 * h, 64 * h + 64)
                i = n * 2 + h
                kb.ts(st["Ysb"][:, n, hc], st["Ysb"][:, n, hc], s1[:, i:i + 1], ALU.subtract, s2[:, i:i + 1], ALU.mult)
            pb = pbs[2 + (n % 2)][0]
            kb.tr(pb[:, 0:64], st["Ysb"][:, n, :], i64)
            kb.ts(st["yT"][:, n * 64:(n + 1) * 64], pb[:, 0:64], col(8), ALU.mult, col(9), ALU.add)
        kb.tt(st["yT"][:, :], st["yT"][:, :], st["bonus"][:, :], ALU.add)
        kb.tt(st["yT"][:, :], st["yT"][:, :], st["gate"][:, :], ALU.mult)
        kb.dma(ctx["ysT_d"][256 + p * 128:256 + (p + 1) * 128, t0:t0 + TB], st["yT"][:, :], w=["ysd"])


TC = 2048
TG = 1024
FG = 256


def emit_norm_F(kb, xb, g_sb, out_bf, sqt, rstd, pbank, ones, eps=EPS, nsub=512, out_f32=None):
    pb = pbank[0]
    for c in range(KC):
        if c % 2 == 0:
            kb.act(sqt[:, c % 4, :], xb[:, c, :], AF.Square)
        else:
            kb.tt(sqt[:, c % 4, :], xb[:, c, :], xb[:, c, :], ALU.mult)
        kb.mm(pb[:, 0:nsub], ones, sqt[:, c % 4, :], start=(c == 0), stop=(c == KC - 1))
    kb.act(rstd[:, :], pb[:, 0:nsub], AF.Sqrt, scale=1.0 / D, bias=eps)
    kb.recip(rstd[:, :], rstd[:, :])
    for c in range(KC):
        kb.stt(out_bf[:, c, :], xb[:, c, :], g_sb[:, c:c + 1], rstd[:, :], ALU.mult, ALU.mult)
        if out_f32 is not None:
            kb.stt(out_f32[:, c, :], xb[:, c, :], g_sb[:, c:c + 1], rstd[:, :], ALU.mult, ALU.mult)


def build_C(kind="dense", final=False, n_groups=None, debug=False, gpe=None):
    kb = KB()
    GPE = gpe if gpe is not None else 7168 // FG
    NGRP = n_groups if n_groups is not None else (5632 // FG if kind == "dense" else 8 * GPE)
    xT_d = kb.din("xT", [D, TC])
    ysT_d = kb.din("ysT", [D, TC])
    cst_d = kb.din("cst", [128, CW])
    vec_d = kb.din("vec", [128, 160])
    wg_d = kb.din("wg", [16, 128, KC * 512])
    bp_d = kb.din("bp", [16, 128, 2048])
    wo_d = kb.din("wo", [16, 128, KC * 128])
    w13_d = kb.din("w13", [NGRP, 128, KC * 2 * FG])
    w2_d = kb.din("w2", [NGRP, 128, 2 * D])
    if kind == "moe":
        rt_d = kb.din("rt", [128, KC * 8])
    out_d = kb.dout("outT", [D, TC])
    h1_d = kb.dout("h1T", [D, TC]) if debug else kb.dscr("h1T", [D, TC])

    cst = kb.sb("cst", [128, CW]); vec = kb.sb("vec", [128, 160])
    kb.dma(cst[:, :], cst_d); kb.dma(vec[:, :], vec_d)
    ones = cst[:, C_ONE:C_ONE + 128]
    ident = cst[:, C_ID:C_ID + 128]
    pbs = [(kb.ps("pb%d" % i), "pb%d" % i) for i in range(8)]
    xv = xT_d.rearrange("(c p) t -> p c t", p=128)
    yv = ysT_d.rearrange("(c p) t -> p c t", p=128)
    h1v = h1_d.rearrange("(c p) t -> p c t", p=128)
    outv = out_d.rearrange("(c p) t -> p c t", p=128)

    kb.phase_begin()
    xb = kb.sb("xb", [128, KC, 512])
    sqt = kb.sb("sqt", [128, 4, 512])
    rstd = kb.sb("rstd", [128, 512])
    hnT = kb.sb("hnT", [128, KC, TG], BF16)
    ysb = kb.sb("ysb", [128, KC, TG], BF16)
    mixT = kb.sb("mixT", [128, KC, TG], BF16)
    wg = [kb.sb("wg%d" % i, [128, KC, 512], BF16) for i in range(2)]
    bpt = [kb.sb("bpt%d" % i, [128, 4, 4, 128], BF16) for i in range(2)]
    wot = [kb.sb("wot%d" % i, [128, KC, 128], BF16) for i in range(2)]
    gsb = [kb.sb("gsb%d" % i, [128, 512]) for i in range(2)]
    macc = kb.sb("macc", [128, 512])
    xres = [kb.sb("xres%d" % i, [128, TG]) for i in range(2)]
    for g2 in range(TC // TG):
        T0 = g2 * TG
        for sub in range(TG // 512):
            kb.dma(xb[:, :, :], xv[:, :, T0 + sub * 512:T0 + (sub + 1) * 512])
            emit_norm_F(kb, xb, vec[:, 0:16], hnT[:, :, sub * 512:(sub + 1) * 512], sqt, rstd, pbs[0], ones)
        for c4 in range(4):
            kb.dma(ysb[:, c4 * 4:(c4 + 1) * 4, :], yv[:, c4 * 4:(c4 + 1) * 4, T0:T0 + TG], eng="gpsimd")
        for dt in range(16):
            wgt = wg[dt % 2]; bp_ = bpt[dt % 2]
            kb.dma(wgt[:, :, :], wg_d[dt].rearrange("p (c n) -> p c n", c=KC), eng="gpsimd")
            kb.dma(bp_[:, :, :, :], bp_d[dt].rearrange("p (i c n) -> p i c n", i=4, c=4), eng="gpsimd")
            for sub in range(TG // 512):
                ts_ = slice(sub * 512, (sub + 1) * 512)
                for i in range(4):
                    pG = pbs[(i % 2)][0]
                    pB = pbs[2 + (i % 2)][0]
                    for c in range(KC):
                        kb.mm(pG[:, :], wgt[:, c, i * 128:(i + 1) * 128], hnT[:, c, ts_], start=(c == 0), stop=(c == KC - 1))
                    for cc in range(4):
                        kb.mm(pB[:, :], bp_[:, i, cc, :], ysb[:, i * 4 + cc, ts_], start=(cc == 0), stop=(cc == 3))
                    gt = gsb[i % 2]
                    kb.act(gt[:, :], pG[:, :], AF.Sigmoid, bias=vec[:, 48 + dt * 4 + i:48 + dt * 4 + i + 1])
                    if i == 0:
                        kb.tt(macc[:, :], gt[:, :], pB[:, :], ALU.mult)
                    else:
                        kb.tt(gt[:, :], gt[:, :], pB[:, :], ALU.mult)
                        kb.tt(macc[:, :], macc[:, :], gt[:, :], ALU.add)
                kb.act(mixT[:, dt, ts_], macc[:, :], AF.Identity)
        for dt in range(16):
            wo_ = wot[dt % 2]
            xr = xres[dt % 2]
            kb.dma(wo_[:, :, :], wo_d[dt].rearrange("p (c n) -> p c n", c=KC), eng="gpsimd")
            kb.dma(xr[:, :], xT_d[dt * 128:(dt + 1) * 128, T0:T0 + TG])
            for sub in range(TG // 512):
                ts_ = slice(sub * 512, (sub + 1) * 512)
                pW = pbs[4 + (sub % 2)][0]
                for c in range(KC):
                    kb.mm(pW[:, :], wo_[:, c, :], mixT[:, c, ts_], start=(c == 0), stop=(c == KC - 1))
                kb.tt(xr[:, ts_], xr[:, ts_], pW[:, :], ALU.add)
            kb.dma(h1_d[dt * 128:(dt + 1) * 128, T0:T0 + TG], xr[:, :], w=[("h1d", dt, g2)])
    kb.phase_end()

    kb.phase_begin()
    acc = kb.sb("acc", [128, KC, TG])
    hn2 = kb.sb("hn2", [128, KC, TG], BF16)
    sqt = kb.sb("sqt2", [128, 4, 512])
    rstd = kb.sb("rstd2", [128, 512])
    w13 = [kb.sb("w13_%d" % i, [128, KC, 2, FG], BF16) for i in range(2)]
    w2t = [kb.sb("w2t_%d" % i, [128, 2, D], BF16) for i in range(2)]
    actT = [kb.sb("actT%d" % i, [128, 2, TG], BF16) for i in range(2)]
    sA = [kb.sb("sA%d" % i, [128, 512]) for i in range(2)]
    if kind == "moe":
        rt = kb.sb("rt", [128, KC, 8])
        kb.dma(rt[:, :, :], rt_d.rearrange("p (c e) -> p c e", c=KC))
        hf = kb.sb("hf", [128, 2, 512])
        lg = kb.sb("lg", [128, 8, 8])
        mx = kb.sb("mx", [128, 8, 8])
        wtk = kb.sb("wtk", [128, 8, 8])
        den = kb.sb("den", [128, 8])
        wT8 = kb.sb("wT8", [8, TG])
        sel = kb.sb("sel", [8, 8, 128])
        wbc = kb.sb("wbc", [128, TG])
        kb.cp(sel[:, :, :], ident[0:8, 0:8].unsqueeze(2).to_broadcast([8, 8, 128]))
    for g2 in range(TC // TG):
        T0 = g2 * TG
        kb.dma(acc[:, :, :], h1v[:, :, T0:T0 + TG], r=[("h1d", dt, g2) for dt in range(16)])
        for sub in range(TG // 512):
            ts_ = slice(sub * 512, (sub + 1) * 512)
            emit_norm_F(kb, acc[:, :, ts_], vec[:, 16:32], hn2[:, :, ts_], sqt, rstd, pbs[0], ones)
            if kind == "moe":
                for c in range(KC):
                    hb = hf[:, c % 2, :]
                    kb.stt(hb, acc[:, c, ts_], vec[:, 16 + c:17 + c], rstd[:, :], ALU.mult, ALU.mult)
                    for j in range(4):
                        kb.mm(pbs[1 + j][0][:, 0:8], hb[:, j * 128:(j + 1) * 128], rt[:, c, :], start=(c == 0), stop=(c == KC - 1))
                for j in range(4):
                    kb.cp(lg[:, sub * 4 + j, :], pbs[1 + j][0][:, 0:8])
        if kind == "moe":
            NTT = TG // 128
            for j in range(NTT):
                kb.P.op("vector", lambda e, o=mx[:, j, :], i=lg[:, j, :]: e.max(out=o, in_=i), reads=kb.ak([lg[:, :, :]], ()), writes=kb.ak([mx[:, :, :]], ()))
            kb.tt(wtk[:, :, :], lg[:, :, :], mx[:, :, 1:2].to_broadcast([128, NTT, 8]), ALU.is_ge)
            kb.tt(lg[:, :, :], lg[:, :, :], mx[:, :, 0:1].to_broadcast([128, NTT, 8]), ALU.subtract)
            kb.act(lg[:, :, :], lg[:, :, :], AF.Exp)
            kb.tt(wtk[:, :, :], wtk[:, :, :], lg[:, :, :], ALU.mult)
            kb.tt(den[:, :], mx[:, :, 1], mx[:, :, 0], ALU.subtract)
            kb.act(den[:, :], den[:, :], AF.Exp)
            kb.ts(den[:, :], den[:, :], 1.0, ALU.add)
            kb.recip(den[:, :], den[:, :])
            kb.tt(wtk[:, :, :], wtk[:, :, :], den[:, :].unsqueeze(2).to_broadcast([128, NTT, 8]), ALU.mult)
            for j in range(NTT):
                kb.tr(pbs[1][0][0:8, j * 128:(j + 1) * 128 - 0][:, 0:128] if False else pbs[1 + j // 4][0][0:8, (j % 4) * 128:(j % 4 + 1) * 128], wtk[:, j, :], ident)
            for q in range(NTT // 4):
                kb.cp(wT8[:, q * 512:(q + 1) * 512], pbs[1 + q][0][0:8, :])
        for grp in range(NGRP):
            wa = w13[grp % 2]; wb = w2t[grp % 2]; at = actT[grp % 2]
            kb.dma(wa[:, :, :, :], w13_d[grp].rearrange("p (c s n) -> p c s n", c=KC, s=2), eng="gpsimd")
            kb.dma(wb[:, :, :], w2_d[grp].rearrange("p (f n) -> p f n", f=2), eng="gpsimd")
            if kind == "moe" and grp % GPE == 0:
                e_ = grp // GPE
                for sub in range(TG // 512):
                    ts_ = slice(sub * 512, (sub + 1) * 512)
                    kb.mm(pbs[7][0][:, :], sel[:, e_, :], wT8[:, ts_])
                    kb.cp(wbc[:, ts_], pbs[7][0][:, :], eng="scalar")
            for sub in range(TG // 512):
                ts_ = slice(sub * 512, (sub + 1) * 512)
                for ft in range(2):
                    pA = pbs[(ft % 2) * 2][0]; pB = pbs[(ft % 2) * 2 + 1][0]
                    for c in range(KC):
                        kb.mm(pA[:, :], wa[:, c, 0, ft * 128:(ft + 1) * 128], hn2[:, c, ts_], start=(c == 0), stop=(c == KC - 1))
                    for c in range(KC):
                        kb.mm(pB[:, :], wa[:, c, 1, ft * 128:(ft + 1) * 128], hn2[:, c, ts_], start=(c == 0), stop=(c == KC - 1))
                    s_ = sA[ft % 2]
                    kb.act(s_[:, :], pA[:, :], AF.Silu)
                    if kind == "moe":
                        kb.tt(s_[:, :], s_[:, :], wbc[:, ts_], ALU.mult)
                    kb.tt(at[:, ft, ts_], s_[:, :], pB[:, :], ALU.mult)
            k = 0
            for dt in range(16):
                for sub in range(TG // 512):
                    ts_ = slice(sub * 512, (sub + 1) * 512)
                    pO = pbs[4 + (k % 4)][0]
                    k += 1
                    for ft in range(2):
                        kb.mm(pO[:, :], wb[:, ft, dt * 128:(dt + 1) * 128], at[:, ft, ts_], start=(ft == 0), stop=(ft == 1))
                    kb.tt(acc[:, dt, ts_], acc[:, dt, ts_], pO[:, :], ALU.add)
        if final:
            for sub in range(TG // 512):
                ts_ = slice(sub * 512, (sub + 1) * 512)
                emit_norm_F(kb, acc[:, :, ts_], vec[:, 32:48], hn2[:, :, ts_], sqt, rstd, pbs[0], ones, out_f32=acc[:, :, ts_])
        kb.dma(outv[:, :, T0:T0 + TG], acc[:, :, :], w=["outd"])
    kb.phase_end()
    return kb.finish()


def prep_C_weights(inp, L, kind, gpe=None, n_groups=None):
    f32 = np.float32
    w_in = np.asarray(inp["w_in"][L])
    wgm = w_in[:, 5912:].reshape(KC, 128, 4, 16, 128)
    wg = np.ascontiguousarray(wgm.transpose(3, 1, 0, 2, 4)).reshape(16, 128, KC * 512)
    bpm = np.asarray(inp["branch_proj"][L]).reshape(4, 4, 128, 16, 128)
    bp = np.ascontiguousarray(bpm.transpose(3, 2, 0, 1, 4)).reshape(16, 128, 2048)
    wom = np.asarray(inp["w_out"][L]).reshape(KC, 128, 16, 128)
    wo = np.ascontiguousarray(wom.transpose(2, 1, 0, 3)).reshape(16, 128, KC * 128)
    vec = np.zeros((128, 160), f32)
    vec[:, 0:16] = np.asarray(inp["norm1_g"][L]).reshape(16, 128).T
    vec[:, 16:32] = np.asarray(inp["norm2_g"][L]).reshape(16, 128).T
    vec[:, 32:48] = np.asarray(inp["final_norm_g"]).reshape(16, 128).T
    gb = np.asarray(inp["gate_bias"][L]).reshape(4, 16, 128)
    vec[:, 48:112] = gb.transpose(2, 1, 0).reshape(128, 64)
    out = dict(wg=wg, bp=bp, wo=wo, vec=vec, cst=make_consts())
    i2 = L // 2
    if kind == "dense":
        w1 = np.asarray(inp["ffn_w1"][i2]); w3 = np.asarray(inp["ffn_w3"][i2]); w2 = np.asarray(inp["ffn_w2"][i2])
        ng = n_groups if n_groups is not None else 5632 // FG
        w13 = np.stack([w1[:, :ng * FG].reshape(KC, 128, ng, FG), w3[:, :ng * FG].reshape(KC, 128, ng, FG)], axis=3)
        out["w13"] = np.ascontiguousarray(w13.transpose(2, 1, 0, 3, 4)).reshape(ng, 128, KC * 2 * FG)
        out["w2"] = np.ascontiguousarray(w2[:ng * FG].reshape(ng, 2, 128, D).transpose(0, 2, 1, 3)).reshape(ng, 128, 2 * D)
    else:
        g_ = gpe if gpe is not None else 7168 // FG
        w1 = np.asarray(inp["moe_w1"][i2]); w3 = np.asarray(inp["moe_w3"][i2]); w2 = np.asarray(inp["moe_w2"][i2])
        w13 = np.stack([w1[:, :, :g_ * FG].reshape(8, KC, 128, g_, FG), w3[:, :, :g_ * FG].reshape(8, KC, 128, g_, FG)], axis=4)
        out["w13"] = np.ascontiguousarray(w13.transpose(0, 3, 2, 1, 4, 5)).reshape(8 * g_, 128, KC * 2 * FG)
        out["w2"] = np.ascontiguousarray(w2[:, :g_ * FG].reshape(8, g_, 2, 128, D).transpose(0, 1, 3, 2, 4)).reshape(8 * g_, 128, 2 * D)
        rt = np.asarray(inp["moe_router"][i2]).reshape(KC, 128, 8)
        out["rt"] = np.ascontiguousarray(rt.transpose(1, 0, 2)).reshape(128, KC * 8)
    return out


_PROGS = {}


def _prog(key, fn):
    if key not in _PROGS:
        _PROGS[key] = fn()
    return _PROGS[key]


def _run_B(inp, L, h_tok):
    nc = _prog("B", lambda: build_B())
    cst = make_consts()
    g1 = np.ascontiguousarray(np.asarray(inp["norm1_g"][L]).reshape(16, 128).T)
    w_in = np.asarray(inp["w_in"][L])
    per_hh = []
    for hh in (0, 1):
        cols = b_cols(hh)
        w = w_in[:, np.maximum(cols, 0)]
        w[:, cols < 0] = 0.0
        per_hh.append(dict(wsel=np.ascontiguousarray(w), prm=make_prm(inp, L, hh), wsm=make_wsm(inp, L, hh)))
    maps = []
    for c in range(8):
        b, hh = c // 2, c % 2
        m = dict(per_hh[hh])
        m.update(x=np.ascontiguousarray(h_tok[b]), g1=g1, cst=cst)
        maps.append(m)
    res = run_bass_kernel_spmd(nc, maps, core_ids=list(range(8)))
    out = []
    for b in range(4):
        full = np.empty((2048, S), np.float32)
        for hh in (0, 1):
            ys = res.results[2 * b + hh]["ysT"]
            for i in range(4):
                full[i * 512 + hh * 256:i * 512 + (hh + 1) * 256] = ys[i * 256:(i + 1) * 256]
        out.append(full)
    return out


def _run_C(inp, L, hT_cores, ys_full, kind, final):
    nc = _prog(("C", kind, final), lambda: build_C(kind, final=final))
    W = prep_C_weights(inp, L, kind)
    maps = []
    for c in range(8):
        b, s = c // 2, c % 2
        m = dict(W)
        m["xT"] = hT_cores[c]
        m["ysT"] = np.ascontiguousarray(ys_full[b][:, s * TC:(s + 1) * TC])
        maps.append(m)
    res = run_bass_kernel_spmd(nc, maps, core_ids=list(range(8)))
    return [res.results[c]["outT"] for c in range(8)]


def kernel(**inputs):
    inp = {k: np.asarray(v) for k, v in inputs.items()}
    x = inp["x"].astype(np.float32, copy=False)
    h_tok = x
    hT = [np.ascontiguousarray(x[c // 2, (c % 2) * TC:(c % 2 + 1) * TC].T) for c in range(8)]
    for L in range(2):
        ys = _run_B(inp, L, h_tok)
        kind = "dense" if L % 2 == 0 else "moe"
        hT = _run_C(inp, L, hT, ys, kind, final=(L == 1))
        if L == 0:
            h_tok = np.stack([np.concatenate([hT[2 * b].T, hT[2 * b + 1].T], axis=0) for b in range(4)], axis=0)
    out = np.stack([np.concatenate([hT[2 * b].T, hT[2 * b + 1].T], axis=0) for b in range(4)], axis=0)
    return np.ascontiguousarray(out.astype(np.float32))
```
